# Optimizing a Trainium2 kernel written in Bass

```python
import math
import jax, jax.numpy as jnp
from jax import lax
import numpy as np

D_MODEL = 1024
BATCH = 4
SEQ = 4096
DEPTH = 4

N_A_LAYERS = DEPTH // 2
N_B_LAYERS = DEPTH - N_A_LAYERS

GLA_HEADS = 4
GLA_KEY_DIM = D_MODEL // 2
GLA_VAL_DIM = D_MODEL
GLA_HK = GLA_KEY_DIM // GLA_HEADS
GLA_HV = GLA_VAL_DIM // GLA_HEADS
GLA_GATE_RANK = 16
GLA_GATE_NORM = 16.0
GLA_CHUNK = 64
A_PROJ = 2 * GLA_KEY_DIM + 2 * GLA_VAL_DIM + GLA_GATE_RANK

SWA_HEAD_DIM = 64
SWA_Q_HEADS = D_MODEL // SWA_HEAD_DIM
SWA_GROUP = 8
SWA_KV_HEADS = SWA_Q_HEADS // SWA_GROUP
WINDOW = 128
BLOCK = 128
KV_PROJ = 2 * SWA_KV_HEADS * SWA_HEAD_DIM

REL_BUCKETS = 32
REL_MAX_DIST = 128

D_FF = 4 * D_MODEL
EPS = 1e-6
NEG = -1e30

kernel_name = "yoco_gla_swa_sink_hybrid"


def rmsnorm(x, g):
    xf = x.astype(jnp.float32)
    y = xf * lax.rsqrt(jnp.mean(xf * xf, axis=-1, keepdims=True) + EPS) * g.astype(jnp.float32)
    return y.astype(x.dtype)


def sq_relu_mlp(h, w_up, w_down):
    u = jax.nn.relu(h @ w_up)
    return (u * u) @ w_down


def gla_mixer(h, w_in, w_gk2, b_gk, g_onorm, w_out):
    B, T, _ = h.shape
    C = GLA_CHUNK
    NC = T // C
    f32 = jnp.float32
    proj = h @ w_in
    q, k, v, gate, glr = jnp.split(
        proj, [GLA_KEY_DIM, 2 * GLA_KEY_DIM, 2 * GLA_KEY_DIM + GLA_VAL_DIM,
               2 * GLA_KEY_DIM + 2 * GLA_VAL_DIM], axis=-1)
    gk = jax.nn.log_sigmoid((glr @ w_gk2 + b_gk).astype(f32)) / GLA_GATE_NORM

    def heads(t, d):
        return t.astype(f32).reshape(B, NC, C, GLA_HEADS, d).transpose(1, 0, 3, 2, 4)

    qf = heads(q, GLA_HK) * (GLA_HK ** -0.5)
    kf = heads(k, GLA_HK)
    vf = heads(v, GLA_HV)
    bcum = jnp.cumsum(heads(gk, GLA_HK), axis=-2)
    q_t = qf * jnp.exp(bcum)
    k_t = kf * jnp.exp(-bcum)
    causal = jnp.asarray(np.tril(np.ones((C, C), dtype=bool)))
    attn = jnp.where(causal, jnp.einsum('nbhik,nbhjk->nbhij', q_t, k_t), 0.0)
    o_intra = jnp.einsum('nbhij,nbhjv->nbhiv', attn, vf)
    b_last = bcum[..., -1:, :]
    k_dec = kf * jnp.exp(b_last - bcum)

    def step(S, xs):
        q_c, k_c, v_c, bl = xs
        o = jnp.einsum('bhik,bhkv->bhiv', q_c, S)
        S = S * jnp.exp(bl)[..., 0, :, None] + jnp.einsum('bhjk,bhjv->bhkv', k_c, v_c)
        return S, o

    S0 = jnp.zeros((B, GLA_HEADS, GLA_HK, GLA_HV), f32)
    _, o_inter = lax.scan(step, S0, (q_t, k_dec, vf, b_last))
    o = (o_intra + o_inter).transpose(1, 0, 3, 2, 4).reshape(B, T, GLA_HEADS, GLA_HV)
    o = o * lax.rsqrt(jnp.mean(o * o, axis=-1, keepdims=True) + EPS) * g_onorm.astype(f32)
    o = o * jax.nn.silu(gate.astype(f32).reshape(B, T, GLA_HEADS, GLA_HV))
    return o.reshape(B, T, GLA_VAL_DIM).astype(h.dtype) @ w_out


def rel_bucket_band():
    i = np.arange(BLOCK)[:, None]
    j = np.arange(2 * BLOCK)[None, :]
    dist = i + BLOCK - j
    n = np.maximum(dist, 0)
    max_exact = REL_BUCKETS // 2
    large = max_exact + (np.log(np.maximum(n, 1) / max_exact)
                         / np.log(REL_MAX_DIST / max_exact)
                         * (REL_BUCKETS - max_exact)).astype(np.int32)
    large = np.minimum(large, REL_BUCKETS - 1)
    bucket = np.where(n < max_exact, n, large).astype(np.int32)
    valid = (dist >= 0) & (dist < WINDOW)
    return bucket, valid


def shared_kv(h, w_kv):
    B, T, _ = h.shape
    NB = T // BLOCK
    k, v = jnp.split(h @ w_kv, 2, axis=-1)

    def band(t):
        t = t.reshape(B, NB, BLOCK, SWA_KV_HEADS, SWA_HEAD_DIM)
        prev = jnp.concatenate([jnp.zeros_like(t[:, :1]), t[:, :-1]], axis=1)
        return jnp.concatenate([prev, t], axis=2)

    return band(k), band(v)


def swa_mixer(h, kb, vb, w_q, sinks, rel_table, w_out):
    B, T, _ = h.shape
    NB = T // BLOCK
    q = (h @ w_q).reshape(B, NB, BLOCK, SWA_KV_HEADS, SWA_GROUP, SWA_HEAD_DIM)
    s = jnp.einsum('bnqhgd,bnkhd->bnhgqk', q, kb).astype(jnp.float32) * (SWA_HEAD_DIM ** -0.5)
    bucket, valid = rel_bucket_band()
    bias = rel_table.astype(jnp.float32)[bucket]
    bias = bias.transpose(2, 0, 1).reshape(SWA_KV_HEADS, SWA_GROUP, BLOCK, 2 * BLOCK)
    first = (np.arange(NB)[:, None] == 0) & (np.arange(2 * BLOCK)[None, :] < BLOCK)
    mask = jnp.asarray(valid[None] & ~first[:, None, :])
    s = jnp.where(mask[None, :, None, None], s + bias, NEG)
    sink = sinks.astype(jnp.float32).reshape(SWA_KV_HEADS, SWA_GROUP, 1, 1)
    m = jnp.maximum(jnp.max(s, axis=-1, keepdims=True), sink)
    p = jnp.exp(s - m)
    p = p / (jnp.sum(p, axis=-1, keepdims=True) + jnp.exp(sink - m))
    o = jnp.einsum('bnhgqk,bnkhd->bnqhgd', p.astype(vb.dtype), vb)
    return o.reshape(B, T, SWA_Q_HEADS * SWA_HEAD_DIM) @ w_out


def setup_inputs(seed: int = 0) -> dict:
    key = jax.random.key(seed)
    ks = jax.random.split(key, 20)
    f32 = jnp.float32

    def w(k, shape, fan_in, scale=1.0):
        return jax.random.normal(k, shape, f32) * (scale * fan_in ** -0.5)

    def gain(k, shape):
        return 1.0 + 0.02 * jax.random.normal(k, shape, f32)

    return {
        "x": jax.random.normal(ks[0], (BATCH, SEQ, D_MODEL), f32),
        "a_w_in": w(ks[1], (N_A_LAYERS, D_MODEL, A_PROJ), D_MODEL),
        "a_w_gk2": w(ks[2], (N_A_LAYERS, GLA_GATE_RANK, GLA_KEY_DIM), GLA_GATE_RANK),
        "a_b_gk": 0.1 * jax.random.normal(ks[3], (N_A_LAYERS, GLA_KEY_DIM), f32),
        "a_onorm": gain(ks[4], (N_A_LAYERS, GLA_HV)),
        "a_w_out": w(ks[5], (N_A_LAYERS, GLA_VAL_DIM, D_MODEL), GLA_VAL_DIM),
        "kv_norm": gain(ks[6], (D_MODEL,)),
        "w_kv": w(ks[7], (D_MODEL, KV_PROJ), D_MODEL),
        "b_w_q": w(ks[8], (N_B_LAYERS, D_MODEL, SWA_Q_HEADS * SWA_HEAD_DIM), D_MODEL),
        "b_sinks": 0.5 * jax.random.normal(ks[9], (N_B_LAYERS, SWA_Q_HEADS), f32),
        "b_w_out": w(ks[10], (N_B_LAYERS, SWA_Q_HEADS * SWA_HEAD_DIM, D_MODEL), D_MODEL),
        "rel_table": 0.5 * jax.random.normal(ks[11], (REL_BUCKETS, SWA_Q_HEADS), f32),
        "ln_mix": gain(ks[12], (DEPTH, D_MODEL)),
        "ln_mlp": gain(ks[13], (DEPTH, D_MODEL)),
        "w_up": w(ks[14], (DEPTH, D_MODEL, D_FF), D_MODEL),
        "w_down": w(ks[15], (DEPTH, D_FF, D_MODEL), D_FF, scale=0.5),
        "ln_final": gain(ks[16], (D_MODEL,)),
    }


def reference(x, a_w_in, a_w_gk2, a_b_gk, a_onorm, a_w_out, kv_norm, w_kv, b_w_q, b_sinks,
              b_w_out, rel_table, ln_mix, ln_mlp, w_up, w_down, ln_final):
    h = x
    kb = vb = None
    for layer in range(DEPTH):
        if layer < N_A_LAYERS:
            h = h + gla_mixer(rmsnorm(h, ln_mix[layer]), a_w_in[layer], a_w_gk2[layer],
                              a_b_gk[layer], a_onorm[layer], a_w_out[layer])
        else:
            if layer == N_A_LAYERS:
                kb, vb = shared_kv(rmsnorm(h, kv_norm), w_kv)
            j = layer - N_A_LAYERS
            h = h + swa_mixer(rmsnorm(h, ln_mix[layer]), kb, vb, b_w_q[j], b_sinks[j],
                              rel_table, b_w_out[j])
        h = h + sq_relu_mlp(rmsnorm(h, ln_mlp[layer]), w_up[layer], w_down[layer])
    return rmsnorm(h, ln_final)
```

```python
import numpy as np
import concourse.bass as bass
import concourse.mybir as mybir
from concourse.bass_utils import run_bass_kernel_spmd

F32 = mybir.dt.float32
BF16 = mybir.dt.bfloat16
AF = mybir.ActivationFunctionType
ALU = mybir.AluOpType
AX = mybir.AxisListType

D = 1024
SEQ = 4096
T = 2048
NG = 4
GS = 512
NT = 16
DFF = 4096
EPS = 1e-6
NEGM = -30000.0
NV = 192
DBG_SWA_LEVEL = 99

class Buf:
    __slots__ = ("name", "w", "r", "sem", "cnt")

    def __init__(self, name):
        self.name = name
        self.w = None
        self.r = {}
        self.sem = None
        self.cnt = 0


class Op:
    __slots__ = ("eng", "fn", "deps", "pos", "inc_ok", "needs_inc", "tok", "dma", "dsem", "dcnt")


ENGS = ("pe", "act", "dve", "pool", "sp")


class Sched:
    def __init__(self):
        self.ops = {e: [] for e in ENGS}
        self.extra = {e: set() for e in ENGS}
        self.dma_bufs = []
        self.all_dma = []
        self.semreg = {}

    def add(self, eng, fn, reads=(), writes=(), inc_ok=True, dma_buf=None, skip_waw=False):
        op = Op()
        op.eng = eng
        op.fn = fn
        op.pos = len(self.ops[eng])
        op.inc_ok = True
        op.needs_inc = False
        op.tok = None
        op.dma = dma_buf is not None
        deps = set(self.extra[eng])
        self.extra[eng] = set()
        for b in reads:
            if b.w is not None:
                deps.add(b.w)
        for b in writes:
            if b.w is not None and not (skip_waw and b is dma_buf):
                deps.add(b.w)
            for r in b.r.values():
                deps.add(r)
        deps.discard(op)
        op.deps = deps
        if op.dma:
            sh = self.semreg.get(dma_buf.name)
            if sh is None:
                sh = Buf("sem:" + dma_buf.name)
                self.semreg[dma_buf.name] = sh
                self.dma_bufs.append(sh)
            sh.cnt += 16
            sh.w = op
            op.dsem = sh
            op.dcnt = sh.cnt
            self.all_dma.append(op)
        for b in writes:
            b.w = op
            b.r = {}
        wset = set(id(b) for b in writes)
        for b in reads:
            if id(b) in wset:
                continue
            key = ("dma", id(op)) if op.dma else eng
            b.r[key] = op
        self.ops[eng].append(op)
        return op

    def barrier(self):
        last = set()
        for e in ENGS:
            if self.ops[e]:
                last.add(self.ops[e][-1])
        for b in self.dma_bufs:
            if b.w is not None and b.w.dma:
                last.add(b.w)
        for op in self.all_dma[-64:]:
            last.add(op)
        for e in ENGS:
            self.extra[e] |= last

    def prepare(self):
        for e in ENGS:
            ops = self.ops[e]
            for op in reversed(ops):
                if not op.dma:
                    op.inc_ok = True
                    break
        nxt = {}
        for e in ENGS:
            ops = self.ops[e]
            arr = [None] * len(ops)
            cur = None
            for i in range(len(ops) - 1, -1, -1):
                if (not ops[i].dma) and ops[i].inc_ok:
                    cur = ops[i]
                arr[i] = cur
            nxt[e] = arr

        def resolve(x):
            if x.dma:
                return x
            r = nxt[x.eng][x.pos]
            assert r is not None, (x.eng, x.pos)
            return r

        for e in ENGS:
            for y in self.ops[e]:
                nd = set()
                for x in y.deps:
                    if (not x.dma) and x.eng == e:
                        if e == "pe":
                            continue
                    rx = resolve(x)
                    if not rx.dma:
                        rx.needs_inc = True
                    nd.add(rx)
                y.deps = nd
        for e in ENGS:
            n = 0
            for op in self.ops[e]:
                if (not op.dma) and op.needs_inc:
                    n += 1
                    op.tok = n

    def emit_engine(self, e, eng, sems, dma_sems):
        known = {}
        nwaits = 0
        for y in self.ops[e]:
            waits = {}
            for x in y.deps:
                if x.dma:
                    k = ("d", id(x.dsem))
                    s = dma_sems[id(x.dsem)]
                    v = x.dcnt
                else:
                    k = ("e", x.eng)
                    s = sems[x.eng]
                    v = x.tok
                if known.get(k, 0) >= v:
                    continue
                if k not in waits or waits[k][1] < v:
                    waits[k] = (s, v)
            for k, (s, v) in waits.items():
                eng.wait_ge(s, v)
                known[k] = v
                nwaits += 1
            ins = y.fn(eng)
            if y.dma:
                ins.then_inc(dma_sems[id(y.dsem)], 16)
            elif y.needs_inc:
                ins.then_inc(sems[e], 1)
        return nwaits


def build_program(nseg=2, stop_after=None, dbg=False):
    nc = bass.Bass("TRN2", target_bir_lowering=False)
    S = Sched()
    ntok = nseg * T

    def din(name, shape):
        return nc.dram_tensor(name, list(shape), F32, kind="ExternalInput").ap()

    x_d = din("x", [ntok, D])
    a_w_in = din("a_w_in", [2, D, 3088])
    a_w_gk2 = din("a_w_gk2", [2, 16, 512])
    a_w_out = din("a_w_out", [2, D, D])
    w_kv = din("w_kv", [D, 256])
    b_w_q = din("b_w_q", [2, D, D])
    b_w_out = din("b_w_out", [2, D, D])
    w_up = din("w_up", [4, D, DFF])
    w_down = din("w_down", [4, DFF, D])
    vecs_d = din("vecs", [128, NV])
    biasg_d = din("biasg", [128, 16 * 256])
    maskneg_d = din("maskneg", [128, 256])
    identf_d = din("identf", [128, 128])
    ones_d = din("onesf", [128, 128])
    mask2_d = din("mask2", [128, 128])
    scanm_d = din("scanm", [128, GS])
    gfin_d = din("gfin", [128, D])
    out_d = nc.dram_tensor("out", [T, D], F32, kind="ExternalOutput").ap()

    from contextlib import ExitStack
    es = ExitStack()

    def sb(name, shape, dt):
        return es.enter_context(nc.sbuf_tensor("s_" + name, list(shape), dt))

    hT = sb("hT", [128, 8, T], F32)
    AT = sb("AT", [128, 8, T], BF16)
    UBN = 36480
    UFN = 6720
    UB = sb("UB", [128, UBN], BF16)
    UF = sb("UF", [128, UFN], F32)
    vecs = sb("vecs", [128, NV], F32)
    nvec = sb("nvec", [128, 104], F32)
    identf = sb("identf", [128, 128], F32)
    cb = sb("cb", [128, 768], BF16)
    mask2 = sb("mask2", [128, 128], F32)
    Rst = sb("Rst", [128, 9, 256], F32)
    elast = sb("elast", [128, 8], F32)
    haloK = sb("haloK", [128, 2, 128], BF16)
    haloV = sb("haloV", [128, 2, 192], BF16)
    print("sbuf bytes remaining:", nc.sbuf_bytes_remaining)

    PS = [es.enter_context(nc.psum_tensor(f"ps{i}", [128, 512], F32)) for i in range(7)]
    PT = es.enter_context(nc.psum_tensor("pst", [128, 1024], BF16))
    PS.append(PT[:, :].bitcast(F32))
    bank = [p[:, :] if not isinstance(p, bass.AP) else p for p in PS]

    hB = [[Buf(f"h{c}_{g}") for g in range(NG)] for c in range(8)]
    AB = [[Buf(f"A{c}_{g}") for g in range(NG)] for c in range(8)]
    PB = [Buf(f"bank{i}") for i in range(8)]
    constB = Buf("consts")
    identb = cb[:, 0:128]
    onesb = cb[:, 128:256]

    def c2d(ap):
        return ap

    def MM(out, lhsT, rhs, start, stop, reads, writes, inc=None):
        inc_ok = stop if inc is None else inc
        return S.add("pe", lambda e: e.matmul(out, lhsT, rhs, start=start, stop=stop),
                     reads=reads, writes=writes, inc_ok=inc_ok)

    def TR(out, in_, ident, reads, writes, inc=True):
        return S.add("pe", lambda e: e.transpose(out, in_, ident), reads=reads, writes=writes, inc_ok=inc)

    def ACT(out, in_, func, reads, writes, bias=None, scale=None, accum=None):
        kw = {}
        if bias is not None:
            kw["bias"] = bias
        if scale is not None:
            kw["scale"] = scale
        if accum is not None:
            kw["accum_out"] = accum
        return S.add("act", lambda e: e.activation(out, in_, func, **kw), reads=reads, writes=writes)

    def TT(eng, out, in0, in1, op, reads, writes):
        return S.add(eng, lambda e: e.tensor_tensor(out, in0, in1, op), reads=reads, writes=writes)

    def TS(eng, out, in0, s1, op0, reads, writes, s2=None, op1=None):
        if op1 is None:
            return S.add(eng, lambda e: e.tensor_scalar(out, in0, s1, None, op0), reads=reads, writes=writes)
        return S.add(eng, lambda e: e.tensor_scalar(out, in0, s1, s2, op0, op1), reads=reads, writes=writes)

    def STT(out, in0, scalar, in1, op0, op1, reads, writes):
        return S.add("dve", lambda e: e.scalar_tensor_tensor(out, in0, scalar, in1, op0, op1),
                     reads=reads, writes=writes)

    def CP(eng, out, in_, reads, writes):
        if eng == "act":
            return S.add("act", lambda e: e.copy(out, in_), reads=reads, writes=writes)
        return S.add(eng, lambda e: e.tensor_copy(out, in_), reads=reads, writes=writes)

    def DMA(q, out, in_, wbuf, reads=(), skip_waw=False, extra_writes=()):
        return S.add(q, lambda e: e.dma_start(out=out, in_=in_), reads=reads,
                     writes=(wbuf,) + tuple(extra_writes), dma_buf=wbuf, skip_waw=skip_waw)

    def MEMSET(eng, ap, val, writes):
        return S.add(eng, lambda e: e.memset(ap, val), writes=writes)

    DMA("sp", vecs[:, :], vecs_d[:, :], constB)
    DMA("sp", identf[:, :], identf_d[:, :], constB, skip_waw=True)
    DMA("sp", mask2[:, :], mask2_d[:, :], constB, skip_waw=True)
    cbB = Buf("cb")
    DMA("pool", cb[:, 0:128], identf_d[:, :], cbB)
    DMA("pool", cb[:, 128:256], ones_d[:, :], cbB, skip_waw=True)
    DMA("pool", cb[:, 256:768], scanm_d[:, :], cbB, skip_waw=True)
    scanm = cb[:, 256:768]
    nvB = Buf("nvec")
    TS("dve", nvec[:, 0:8], vecs[:, 80:88], -1.0, ALU.mult, [constB], [nvB])
    TS("dve", nvec[:, 8:40], vecs[:, 92:124], -1.0, ALU.mult, [constB], [nvB])
    TS("dve", nvec[:, 40:104], vecs[:, 128:192], -1.0, ALU.mult, [constB], [nvB])
    stB = Buf("state")
    MEMSET("dve", Rst[:, :, :], 0.0, [stB])
    MEMSET("dve", elast[:, :], 1.0, [stB])
    hkB = Buf("haloK")
    hvB = Buf("haloV")
    MEMSET("dve", haloK[:, :, :], 0.0, [hkB])
    MEMSET("dve", haloV[:, :, :], 0.0, [hvB])

    def gcol(i):
        return vecs[:, i:i + 1]

    class Carver:
        def __init__(self, t, n):
            self.t = t
            self.n = n
            self.off = 0

        def take(self, n, shape=None):
            a = self.t[:, self.off:self.off + n]
            self.off += n
            assert self.off <= self.n, (self.off, self.n)
            if shape is not None:
                if len(shape) == 2:
                    a = a.rearrange("p (a b) -> p a b", a=shape[0], b=shape[1])
                elif len(shape) == 3:
                    a = a.rearrange("p (a b c) -> p a b c", a=shape[0], b=shape[1], c=shape[2])
            return a

    def rmsnorm(cf, cbf, gbase, groups=None):
        nsq = cbf.take(2 * GS, (2, GS))
        nln = cf.take(GS)
        nrs = cf.take(2 * GS, (2, GS))
        sqB = [Buf("nsq0"), Buf("nsq1")]
        lnB = Buf("nln")
        rsB = [Buf("nrs0"), Buf("nrs1")]
        for g in (range(NG) if groups is None else groups):
            pb = 0
            for c in range(8):
                j = c % 2
                ACT(nsq[:, j, :], hT[:, c, g * GS:(g + 1) * GS], AF.Square, [hB[c][g]], [sqB[j]])
                MM(bank[pb], onesb, nsq[:, j, :], c == 0, c == 7, [sqB[j], cbB], [PB[pb]])
            ACT(nln[:, :], bank[pb], AF.Ln, [PB[pb]], [lnB], bias=EPS, scale=1.0 / D)
            r = g % 2
            ACT(nrs[:, r, :], nln[:, :], AF.Exp, [lnB], [rsB[r]], scale=-0.5)
            for c in range(8):
                STT(AT[:, c, g * GS:(g + 1) * GS], hT[:, c, g * GS:(g + 1) * GS], gcol(gbase + c),
                    nrs[:, r, :], ALU.mult, ALU.mult, [hB[c][g], rsB[r], constB], [AB[c][g]])

    def load_segment(s):
        cf = Carver(UF, UFN)
        xin = cf.take(2 * D, (2, D))
        xB = [Buf("xin0"), Buf("xin1")]
        for t in range(NT):
            j = t % 2
            g = t // 4
            DMA("sp", xin[:, j, :], x_d[s * T + t * 128: s * T + (t + 1) * 128, :], xB[j])
            for half in range(2):
                pb = 2 * j + half
                for cc in range(4):
                    c = half * 4 + cc
                    TR(bank[pb][:, cc * 128:(cc + 1) * 128], xin[:, j, c * 128:(c + 1) * 128], identf[:, :],
                       [xB[j], constB], [PB[pb]], inc=(cc == 3))
                dst = hT[:, half * 4:half * 4 + 4, t * 128:(t + 1) * 128]
                src = bank[pb].rearrange("p (a b) -> p a b", a=4, b=128)
                eng = "act" if half == 0 else "dve"
                CP(eng, dst, src, [PB[pb]], [hB[half * 4 + cc][g] for cc in range(4)])

    def wload(dst, src, wbuf, first=True):
        DMA("pool", dst, src, wbuf, skip_waw=not first)

    def rows_view(w2d):
        return w2d.rearrange("(kc p) f -> p kc f", p=128)

    def mlp(l, s, preserve_kv=False, groups=None):
        groups = list(range(NG)) if groups is None else list(groups)
        cf = Carver(UF, UF_LO if preserve_kv else UFN)
        cbf = Carver(UB, UB_LO if preserve_kv else UBN)
        rmsnorm(cf, cbf, 32 + l * 8, groups)
        rt = cf.take(2 * GS, (2, GS))
        rtB = [Buf("rt0"), Buf("rt1")]
        u2 = cbf.take(4 * T, (4, T))
        u2B = [[Buf(f"u2_{c}_{g}") for g in range(NG)] for c in range(4)]
        wu = [cbf.take(8 * 512, (8, 512)) for _ in range(2)]
        wd = [cbf.take(4 * 1024, (4, 1024)) for _ in range(2)]
        wuB = [Buf("wu0"), Buf("wu1")]
        wdB = [Buf("wd0"), Buf("wd1")]
        wupv = rows_view(w_up[l])
        wdnv = rows_view(w_down[l])
        NF = DFF // 512

        def load(f):
            j = f % 2
            wload(wu[j], wupv[:, :, f * 512:(f + 1) * 512], wuB[j])
            wload(wd[j], wdnv[:, f * 4:(f + 1) * 4, :], wdB[j])

        load(0)
        setc = 0
        ri = 0
        for f in range(NF):
            j = f % 2
            if f + 1 < NF:
                load(f + 1)
            for fc in range(4):
                bs = (setc % 2) * 4
                setc += 1
                for kc in range(8):
                    for g in groups:
                        MM(bank[bs + g], wu[j][:, kc, fc * 128:(fc + 1) * 128], AT[:, kc, g * GS:(g + 1) * GS],
                           kc == 0, kc == 7, [wuB[j], AB[kc][g]], [PB[bs + g]])
                for g in groups:
                    r = ri % 2
                    ri += 1
                    ACT(rt[:, r, :], bank[bs + g], AF.Relu, [PB[bs + g]], [rtB[r]])
                    TT("dve", u2[:, fc, g * GS:(g + 1) * GS], rt[:, r, :], rt[:, r, :], ALU.mult,
                       [rtB[r]], [u2B[fc][g]])
            for oc in range(8):
                bs = (setc % 2) * 4
                setc += 1
                for k2 in range(4):
                    for g in groups:
                        MM(bank[bs + g], wd[j][:, k2, oc * 128:(oc + 1) * 128], u2[:, k2, g * GS:(g + 1) * GS],
                           k2 == 0, k2 == 3, [wdB[j], u2B[k2][g]], [PB[bs + g]])
                for g in groups:
                    hv = hT[:, oc, g * GS:(g + 1) * GS]
                    TT("dve", hv, bank[bs + g], hv, ALU.add, [PB[bs + g], hB[oc][g]], [hB[oc][g]])

    def gla(l, s, out_groups=None):
        out_groups = set(range(NG)) if out_groups is None else set(out_groups)
        cf = Carver(UF, UFN)
        cbf = Carver(UB, UBN)
        rmsnorm(cf, cbf, l * 8)
        tE = cf.take(GS)
        tL = cf.take(GS)
        Lc = cf.take(GS)
        Ae = [cf.take(GS) for _ in range(2)]
        Ai = cf.take(GS)
        rstd = cf.take(GS)
        lnv = cf.take(GS)
        sgr = cf.take(2 * GS, (2, GS))
        tEB, tLB, LcB, AiB, rsB, lnB = (Buf(n) for n in ("tE", "tL", "Lc", "Ai", "grs", "gln"))
        AeB = [Buf("Ae0"), Buf("Ae1")]
        sgrB = [Buf("sgr0"), Buf("sgr1")]
        glrT = cbf.take(T)
        wgk = cbf.take(512)
        wglr = cbf.take(8 * 16, (8, 16))
        qt = [cbf.take(GS) for _ in range(2)]
        kt = [cbf.take(GS) for _ in range(2)]
        vtok = [cbf.take(4 * 256, (4, 256)) for _ in range(2)]
        ktok = [cbf.take(4 * 128, (4, 128)) for _ in range(2)]
        sg = [cbf.take(2 * GS, (2, GS)) for _ in range(2)]
        attm = cbf.take(2 * 128, (2, 128))
        Sbf = cbf.take(8 * 256, (8, 256))
        sq = cbf.take(2 * GS, (2, GS))
        og = [cbf.take(2 * GS, (2, GS)) for _ in range(2)]
        wqkv = [cbf.take(8 * 512, (8, 512)) for _ in range(2)]
        wg = [cbf.take(8 * 256, (8, 256)) for _ in range(2)]
        wo = [cbf.take(2 * 1024, (2, 1024)) for _ in range(2)]
        glrB, wgkB, wglrB = (Buf(n) for n in ("glrT", "wgk", "wglr"))
        qtB = [Buf("qt0"), Buf("qt1")]
        ktB = [Buf("kt0"), Buf("kt1")]
        vtB = [Buf("vtok0"), Buf("vtok1")]
        ktkB = [Buf("ktok0"), Buf("ktok1")]
        sgB = [[Buf(f"sg{p}_{v}") for v in range(2)] for p in range(2)]
        atB = [Buf("att0"), Buf("att1")]
        SbB = [Buf(f"Sbf{c}") for c in range(8)]
        sqB = [Buf("gsq0"), Buf("gsq1")]
        ogB = [[Buf(f"og{p}_{v}") for v in range(2)] for p in range(2)]
        wqkvB = [Buf("wqkv0"), Buf("wqkv1")]
        wgB = [Buf("wg0"), Buf("wg1")]
        woB = [Buf("wo0"), Buf("wo1")]
        RB = [[Buf(f"R{h}_{p}") for p in range(2)] for h in range(4)]
        elB = [Buf(f"el{h}") for h in range(4)]
        for h in range(4):
            for p in range(2):
                RB[h][p].w = stB.w
            elB[h].w = stB.w

        win = rows_view(a_w_in[l])
        wov = rows_view(a_w_out[l])

        wload(wgk[0:16, :], a_w_gk2[l], wgkB)
        wload(wglr, win[:, :, 3072:3088], wglrB)

        def load_head(h):
            j = h % 2
            wload(wqkv[j][:, :, 0:128], win[:, :, h * 128:(h + 1) * 128], wqkvB[j])
            wload(wqkv[j][:, :, 128:256], win[:, :, 512 + h * 128:512 + (h + 1) * 128], wqkvB[j], first=False)
            wload(wqkv[j][:, :, 256:512], win[:, :, 1024 + h * 256:1024 + (h + 1) * 256], wqkvB[j], first=False)
            wload(wg[j], win[:, :, 2048 + h * 256:2048 + (h + 1) * 256], wgB[j])

        def load_wo(h):
            wload(wo[h % 2], wov[:, 2 * h:2 * h + 2, :], woB[h % 2])

        load_head(0)
        load_head(1)
        load_wo(0)
        load_wo(1)
        for g in range(NG):
            for kc in range(8):
                MM(bank[g][0:16, :], wglr[:, kc, :], AT[:, kc, g * GS:(g + 1) * GS], kc == 0, kc == 7,
                   [wglrB, AB[kc][g]], [PB[g]])
            CP("act", glrT[0:16, g * GS:(g + 1) * GS], bank[g][0:16, :], [PB[g]], [glrB])

        units = [(h, g) for h in range(4) for g in range(NG)]
        NU = len(units)
        rp = [0, 0, 0, 0]
        oset = [0]

        def G0(u):
            h, g = units[u]
            p = u % 2
            st = l * 4 + h
            gs = slice(g * GS, (g + 1) * GS)
            MM(bank[0], wgk[0:16, h * 128:(h + 1) * 128], glrT[0:16, gs], True, True, [wgkB, glrB], [PB[0]])
            ACT(tE, bank[0], AF.Exp, [PB[0], nvB], [tEB], bias=nvec[:, st:st + 1], scale=-1.0)
            ACT(tL, tE, AF.Ln, [tEB], [tLB], bias=1.0)
            S.add("dve", lambda e, o=Lc, a_=scanm, b_=tL: e.tensor_tensor_scan(o, a_, b_, 0.0, ALU.mult, ALU.add),
                  reads=[tLB, cbB], writes=[LcB])
            ACT(Ae[p], Lc, AF.Exp, [LcB], [AeB[p]], scale=-1.0 / 16.0)
            ACT(Ai, Lc, AF.Exp, [LcB], [AiB], scale=1.0 / 16.0)

        def G1(u):
            h, g = units[u]
            p = u % 2
            j = h % 2
            gs = slice(g * GS, (g + 1) * GS)
            if g == 0 and h >= 1 and h + 1 < 4:
                load_head(h + 1)
            need_out = g in out_groups
            if need_out:
                for kc in range(8):
                    MM(bank[1], wqkv[j][:, kc, 0:128], AT[:, kc, gs], kc == 0, kc == 7, [wqkvB[j], AB[kc][g]], [PB[1]])
                STT(qt[p], bank[1], float(128 ** -0.5), Ae[p], ALU.mult, ALU.mult, [PB[1], AeB[p]], [qtB[p]])
                yield
            for kc in range(8):
                MM(bank[0], wqkv[j][:, kc, 128:256], AT[:, kc, gs], kc == 0, kc == 7, [wqkvB[j], AB[kc][g]], [PB[0]])
            TT("dve", kt[p], bank[0], Ai, ALU.mult, [PB[0], AiB], [ktB[p]])
            yield
            for half in range(2):
                pb = 1 - half
                for t2 in range(2):
                    tt = half * 2 + t2
                    for kc in range(8):
                        MM(bank[pb][:, t2 * 256:(t2 + 1) * 256],
                           AT[:, kc, g * GS + tt * 128:g * GS + (tt + 1) * 128], wqkv[j][:, kc, 256:512],
                           kc == 0, kc == 7, [wqkvB[j], AB[kc][g]], [PB[pb]])
                CP("act", vtok[p][:, 2 * half:2 * half + 2, :],
                   bank[pb].rearrange("p (a b) -> p a b", a=2, b=256), [PB[pb]], [vtB[p]])
                yield
            for vc in range(2):
                if not need_out:
                    break
                pb = 1 - vc
                for kc in range(8):
                    MM(bank[pb], wg[j][:, kc, vc * 128:(vc + 1) * 128], AT[:, kc, gs], kc == 0, kc == 7,
                       [wgB[j], AB[kc][g]], [PB[pb]])
                ACT(sg[p][:, vc, :], bank[pb], AF.Silu, [PB[pb]], [sgB[p][vc]])
                yield
            b1h = bank[1].bitcast(BF16)
            for tt in range(4):
                TR(b1h[:, tt * 128:(tt + 1) * 128], kt[p][:, tt * 128:(tt + 1) * 128], identb, [ktB[p], cbB], [PB[1]])
            CP("dve", ktok[p], b1h[:, 0:512].rearrange("p (a b) -> p a b", a=4, b=128), [PB[1]], [ktkB[p]])

        def G2(u):
            h, g = units[u]
            p = u % 2
            st = l * 4 + h
            R = [Rst[:, st, :], Rst[:, 8, :]]
            el = elast[:, st:st + 1]
            for c in range(8):
                tt = c // 2
                r0 = (c % 2) * 64
                pb = 2 + (c % 2)
                MM(bank[pb][:, 0:256], ktok[p][r0:r0 + 64, tt, :], vtok[p][r0:r0 + 64, tt, :], True, True,
                   [ktkB[p], vtB[p]], [PB[pb]])
                eprev = el if c == 0 else Ae[p][:, c * 64 - 1:c * 64]
                erd = [elB[h]] if c == 0 else [AeB[p]]
                r_ = rp[h]
                if g in out_groups:
                    TS("dve", Sbf[:, c, :], R[r_], eprev, ALU.mult, [RB[h][r_]] + erd, [SbB[c]])
                STT(R[1 - r_], R[r_], eprev, bank[pb][:, 0:256], ALU.mult, ALU.add,
                    [RB[h][r_], PB[pb]] + erd, [RB[h][1 - r_]])
                rp[h] = 1 - r_
                if c % 2 == 1:
                    yield
            CP("dve", el, Ae[p][:, GS - 1:GS], [AeB[p]], [elB[h]])
            for tt in range(4):
                if g not in out_groups:
                    break
                ts_ = slice(tt * 128, (tt + 1) * 128)
                MM(bank[4][:, 0:128], kt[p][:, ts_], qt[p][:, ts_], True, True, [ktB[p], qtB[p]], [PB[4]])
                TT("dve", attm[:, tt % 2, :], bank[4][:, 0:128], mask2[:, :], ALU.mult, [PB[4], constB], [atB[tt % 2]])
                for vc in range(2):
                    po = 5 + vc
                    MM(bank[po][:, ts_], vtok[p][:, tt, vc * 128:(vc + 1) * 128], attm[:, tt % 2, :], True, False,
                       [vtB[p], atB[tt % 2]], [PB[po]])
                    for hf in range(2):
                        c = 2 * tt + hf
                        cs = slice(tt * 128 + hf * 64, tt * 128 + hf * 64 + 64)
                        MM(bank[po][:, cs], Sbf[:, c, vc * 128:(vc + 1) * 128], qt[p][:, cs], False, hf == 1,
                           [SbB[c], qtB[p]], [PB[po]])
                yield

        def G3(u):
            h, g = units[u]
            if g not in out_groups:
                return
            p = u % 2
            for vc in range(2):
                ACT(sq[:, vc, :], bank[5 + vc], AF.Square, [PB[5 + vc]], [sqB[vc]])
                MM(bank[2], onesb, sq[:, vc, :], vc == 0, vc == 1, [sqB[vc], cbB], [PB[2]])
            ACT(lnv, bank[2], AF.Ln, [PB[2]], [lnB], bias=EPS, scale=1.0 / 256.0)
            ACT(rstd, lnv, AF.Exp, [lnB], [rsB], scale=-0.5)
            for vc in range(2):
                TT("dve", sgr[:, vc, :], sg[p][:, vc, :], rstd, ALU.mult, [sgB[p][vc], rsB], [sgrB[vc]])
                STT(og[p][:, vc, :], bank[5 + vc], gcol(88 + l * 2 + vc), sgr[:, vc, :], ALU.mult, ALU.mult,
                    [PB[5 + vc], sgrB[vc], constB], [ogB[p][vc]])

        def G4(u):
            h, g = units[u]
            p = u % 2
            j = h % 2
            for oc in range(8):
                if g not in out_groups:
                    break
                pb = 7 if oset[0] % 2 == 0 else 4
                oset[0] += 1
                for k2 in range(2):
                    MM(bank[pb], wo[j][:, k2, oc * 128:(oc + 1) * 128], og[p][:, k2, :], k2 == 0, k2 == 1,
                       [woB[j], ogB[p][k2]], [PB[pb]])
                hv = hT[:, oc, g * GS:(g + 1) * GS]
                TT("dve", hv, bank[pb], hv, ALU.add, [PB[pb], hB[oc][g]], [hB[oc][g]])
                yield
            if g == NG - 1 and h + 2 < 4:
                load_wo(h + 2)

        def drain(gen):
            for _ in gen:
                pass

        for k in range(NU + 5):
            if 0 <= k - 3 < NU:
                G3(k - 3)
            g4 = G4(k - 4) if 0 <= k - 4 < NU else iter(())
            g2 = G2(k - 2) if 0 <= k - 2 < NU else iter(())
            g1 = G1(k - 1) if 0 <= k - 1 < NU else iter(())
            alive = True
            while alive:
                alive = False
                for gen in (g4, g2, g1):
                    try:
                        next(gen)
                        alive = True
                    except StopIteration:
                        pass
            if 0 <= k < NU:
                G0(k)

    KV_ELEMS = 2 * 17 * 128 + 17 * 2 * 192
    UB_LO = UBN - KV_ELEMS
    UF_LO = UFN - 16 * 256
    swa_ctx = {}

    def kv_phase(s, groups=None):
        groups = list(range(NG)) if groups is None else list(groups)
        cf = Carver(UF, UF_LO)
        cbf = Carver(UB, UB_LO)
        rmsnorm(cf, cbf, 64, groups)
        top = Carver(UB, UBN)
        top.off = UB_LO
        KT = top.take(2 * 17 * 128, (2, 17 * 128))
        Vp = top.take(17 * 2 * 192, (17, 2, 192))
        ftop = Carver(UF, UFN)
        ftop.off = UF_LO
        biasT = ftop.take(16 * 256, (16, 256))
        biasB = Buf("biasT")
        KTB = [Buf("KT_halo")] + [Buf(f"KT{g}") for g in range(NG)]
        VpB = [Buf("Vp_halo")] + [Buf(f"Vp{g}") for g in range(NG)]
        swa_ctx.update(KT=KT, Vp=Vp, biasT=biasT, biasB=biasB, KTB=KTB, VpB=VpB)
        mk = cf.take(256)
        mkB = Buf("mk")
        DMA("sp", biasT.rearrange("p a b -> p (a b)"), biasg_d[:, :], biasB)
        DMA("sp", mk, maskneg_d[:, :], mkB)
        for hh in range(16):
            TT("dve", biasT[:, hh, :], biasT[:, hh, :], mk, ALU.add, [biasB, mkB], [biasB])
        wkd = cbf.take(8 * 256, (8, 2, 128))
        wvv = cbf.take(8 * 128, (8, 128))
        wkB, wvB = Buf("wkd"), Buf("wvv")
        wkvv = rows_view(w_kv)
        first = True
        for gk in range(2):
            for d2 in range(2):
                wload(wkd[:, :, gk, d2 * 64:(d2 + 1) * 64], wkvv[:, :, gk * 64:(gk + 1) * 64], wkB, first=first)
                first = False
        wload(wvv, wkvv[:, :, 128:256], wvB)
        MEMSET("pool", Vp[:, :, :, :], 0.0, VpB)
        CP("pool", KT[:, :, 0:128], haloK[:, :, :], [hkB], [KTB[0]])
        CP("pool", Vp[:, 0, :, :], haloV[:, :, :], [hvB], [VpB[0]])
        for gk in range(2):
            for kc in range(8):
                for g in groups:
                    MM(bank[g], wkd[:, kc, gk, :], AT[:, kc, g * GS:(g + 1) * GS], kc == 0, kc == 7,
                       [wkB, AB[kc][g]], [PB[g]])
            for g in groups:
                CP("act", KT[:, gk, 128 + g * GS:128 + (g + 1) * GS], bank[g], [PB[g]], [KTB[1 + g]])
        for g in groups:
            pb = 4 + (g % 2)
            for tt in range(4):
                for kc in range(8):
                    MM(bank[pb][:, tt * 128:(tt + 1) * 128], AT[:, kc, g * GS + tt * 128:g * GS + (tt + 1) * 128],
                       wvv[:, kc, :], kc == 0, kc == 7, [wvB, AB[kc][g]], [PB[pb]], inc=(kc == 7 and tt == 3))
            CP("act", Vp[:, 1 + 4 * g:5 + 4 * g, :, 64:128],
               bank[pb].rearrange("p (a b c) -> p a b c", a=4, b=2, c=64), [PB[pb]], [VpB[1 + g]])
        CP("pool", haloK[:, :, :], KT[:, :, 16 * 128:17 * 128], [KTB[NG]], [hkB])
        CP("pool", haloV[:, :, :], Vp[:, 16, :, :], [VpB[NG]], [hvB])

    def swa_layer(l, s):
        jj = l - 2
        KT, Vp, biasT, biasB = swa_ctx["KT"], swa_ctx["Vp"], swa_ctx["biasT"], swa_ctx["biasB"]
        KTB, VpB = swa_ctx["KTB"], swa_ctx["VpB"]
        cf = Carver(UF, UF_LO)
        cbf = Carver(UB, UB_LO)
        rmsnorm(cf, cbf, l * 8)
        S.barrier()
        cf = Carver(UF, UF_LO)
        cbf = Carver(UB, UB_LO)
        ssb = [cf.take(1024, (2, 2, 256)) for _ in range(2)]
        stat = [cf.take(32) for _ in range(4)]
        Wq = cbf.take(8 * 1024, (8, 1024))
        Wo = cbf.take(8 * 1024, (8, 1024))
        qh = cbf.take(2 * GS, (2, GS))
        oTg = cbf.take(8 * GS, (8, GS))
        pbuf = [cbf.take(1024, (2, 2, 256)) for _ in range(2)]
        dg = [cbf.take(512, (4, 128)) for _ in range(2)]
        pnT = cbf.take(1024)
        ssB = [Buf("ss0"), Buf("ss1")]
        WqB, WoB = Buf("Wq"), Buf("Wo")
        qhB = [Buf("qh0"), Buf("qh1")]
        oTB = [Buf(f"oT{c}") for c in range(8)]
        pB = [Buf("p0"), Buf("p1")]
        pnB = Buf("pn")
        dgB = [Buf("dg0"), Buf("dg1")]
        stB_ = [Buf(f"stat{i}") for i in range(4)]
        for hf in range(2):
            wload(Wq[:, :, hf * 512:(hf + 1) * 512], rows_view(b_w_q[jj])[:, :, hf * 512:(hf + 1) * 512], WqB, first=(hf == 0))
        for hf in range(2):
            wload(Wo[:, :, hf * 512:(hf + 1) * 512], rows_view(b_w_out[jj])[:, :, hf * 512:(hf + 1) * 512], WoB, first=(hf == 0))
        units = [(g, hp, su) for g in range(NG) for hp in range(8) for su in range(2)]
        NU = len(units)
        BQ, BO = 7, 6

        def ktb(t):
            r = [KTB[1 + (t // 4)]]
            r.append(KTB[0] if t == 0 else KTB[1 + ((t - 1) // 4)])
            return r

        def vpb(i):
            return VpB[0] if i == 0 else VpB[1 + (i - 1) // 4]

        def st_q(u):
            g, hp, su = units[u]
            if su != 0:
                return
            qb = hp % 2
            for kc in range(8):
                MM(bank[BQ], Wq[:, kc, hp * 128:(hp + 1) * 128], AT[:, kc, g * GS:(g + 1) * GS],
                   kc == 0, kc == 7, [WqB, AB[kc][g]], [PB[BQ]])
            S.add("act", lambda e, o=qh[:, qb, :], i=bank[BQ]: e.mul(o, i, 0.125), reads=[PB[BQ]], writes=[qhB[qb]])

        def st0(u):
            g, hp, su = units[u]
            x = u % 2
            gk = hp // 4
            qb = hp % 2
            for ti in range(2):
                tt = 2 * su + ti
                t = g * 4 + tt
                for a in range(2):
                    r0 = a * 64
                    MM(bank[2 * x + a][:, ti * 256:(ti + 1) * 256], qh[r0:r0 + 64, qb, tt * 128:(tt + 1) * 128],
                       KT[r0:r0 + 64, gk, t * 128:(t + 2) * 128], True, True, [qhB[qb]] + ktb(t), [PB[2 * x + a]])

        def st1(u):
            g, hp, su = units[u]
            x = u % 2
            for a in range(2):
                for ti in range(2):
                    TT("dve", ssb[x][:, a, ti, :], bank[2 * x + a][:, ti * 256:(ti + 1) * 256],
                       biasT[:, 2 * hp + a, :], ALU.add, [PB[2 * x + a], biasB], [ssB[x]])
            if g == 0 and su == 0:
                for a in range(2):
                    TS("dve", ssb[x][:, a, 0, 0:128], ssb[x][:, a, 0, 0:128], vecs[:, 124:125], ALU.add,
                       [ssB[x], constB], [ssB[x]])
            y = u % 4
            st_ = stat[y]
            S.add("dve", lambda e, o=st_[:, 0:4], i=ssb[x].rearrange("p a t k -> p (a t) k"):
                  e.tensor_reduce(o, i, AX.X, ALU.max), reads=[ssB[x]], writes=[stB_[y]])
            nsk4 = nvec[:, 40 + jj * 32 + hp * 4:40 + jj * 32 + hp * 4 + 4]
            STT(st_[:, 4:8], st_[:, 0:4], -1.0, nsk4, ALU.mult, ALU.min, [stB_[y], nvB], [stB_[y]])
            TT("dve", st_[:, 12:16], st_[:, 4:8], nsk4, ALU.subtract, [stB_[y], nvB], [stB_[y]])

        def st2(u):
            x = u % 2
            y = u % 4
            st_ = stat[y]
            for a in range(2):
                for ti in range(2):
                    k = a * 2 + ti
                    ACT(pbuf[x][:, a, ti, :], ssb[x][:, a, ti, :], AF.Exp, [ssB[x], stB_[y]], [pB[x], stB_[y]],
                        bias=st_[:, 4 + k:5 + k], scale=1.0, accum=st_[:, 8 + k:9 + k])
            ACT(st_[:, 16:20], st_[:, 12:16], AF.Exp, [stB_[y]], [stB_[y]])

        def st3(u):
            x = u % 2
            y = u % 4
            st_ = stat[y]
            TT("dve", st_[:, 20:24], st_[:, 8:12], st_[:, 16:20], ALU.add, [stB_[y]], [stB_[y]])
            S.add("dve", lambda e, o=st_[:, 24:28], i=st_[:, 20:24]: e.reciprocal(o, i), reads=[stB_[y]], writes=[stB_[y]])
            for k in range(4):
                TS("dve", dg[x][:, k, :], identb, st_[:, 24 + k:25 + k], ALU.mult, [cbB, stB_[y]], [dgB[x]])

        def st4(u):
            x = u % 2
            for a in range(2):
                for ti in range(2):
                    k = a * 2 + ti
                    for jt in range(2):
                        idx = k * 2 + jt
                        pb = 4 + idx // 4
                        MM(bank[pb][:, (idx % 4) * 128:(idx % 4 + 1) * 128], pbuf[x][:, a, ti, jt * 128:(jt + 1) * 128],
                           dg[x][:, k, :], True, True, [pB[x], dgB[x]], [PB[pb]])

        def st5(u):
            CP("act", pnT[:, 0:512], bank[4], [PB[4]], [pnB])
            CP("act", pnT[:, 512:1024], bank[5], [PB[5]], [pnB])

        def st6(u):
            g, hp, su = units[u]
            gk = hp // 4
            for ti in range(2):
                tt = 2 * su + ti
                t = g * 4 + tt
                ia = 0
                for a in range(2):
                    k = a * 2 + ti
                    for jt in range(2):
                        idx = k * 2 + jt
                        vi = t + jt
                        lhs = Vp[:, vi, gk, 64:192] if a == 0 else Vp[:, vi, gk, 0:128]
                        MM(bank[BO][:, tt * 128:(tt + 1) * 128], lhs, pnT[:, idx * 128:(idx + 1) * 128],
                           ia == 0, ia == 3, [vpb(vi), pnB], [PB[BO]])
                        ia += 1
            if su == 1:
                CP("act", oTg[:, hp, :], bank[BO], [PB[BO]], [oTB[hp]])
                if hp == 7:
                    for oc in range(8):
                        pb = BQ if oc % 2 == 0 else BO
                        for kc in range(8):
                            MM(bank[pb], Wo[:, kc, oc * 128:(oc + 1) * 128], oTg[:, kc, :], kc == 0, kc == 7,
                               [WoB, oTB[kc]], [PB[pb]])
                        hv = hT[:, oc, g * GS:(g + 1) * GS]
                        TT("dve", hv, bank[pb], hv, ALU.add, [PB[pb], hB[oc][g]], [hB[oc][g]])

        stages = [st0, st1, st2, st3, st4, st5, st6]
        st_q(0)
        for k in range(-2, NU + len(stages)):
            for si in reversed(range(len(stages))):
                u = k - si
                if 0 <= u < NU:
                    stages[si](u)
            if 0 <= k + 2 < NU and k + 2 > 0:
                st_q(k + 2)

    def final_store(s, with_norm=True):
        cf = Carver(UF, UFN)
        gfin = cf.take(D)
        ost = cf.take(2 * D, (2, D))
        stat = cf.take(16)
        gfB = Buf("gfin")
        osB = [Buf("ost0"), Buf("ost1")]
        fsB = Buf("fstat")
        DMA("sp", gfin, gfin_d[:, :], gfB)
        for t in range(NT):
            g = t // 4
            j = t % 2
            for half in range(2):
                pb = 2 * j + half
                for cc in range(4):
                    c = half * 4 + cc
                    TR(bank[pb][:, cc * 128:(cc + 1) * 128], hT[:, c, t * 128:(t + 1) * 128], identf[:, :],
                       [hB[c][g], constB], [PB[pb]], inc=(cc == 3))
            if with_norm:
                for half in range(2):
                    pb = 2 * j + half
                    ACT(ost[:, j, half * 512:(half + 1) * 512], bank[pb], AF.Square, [PB[pb]], [osB[j], fsB],
                        accum=stat[:, half:half + 1])
                TT("dve", stat[:, 2:3], stat[:, 0:1], stat[:, 1:2], ALU.add, [fsB], [fsB])
                ACT(stat[:, 3:4], stat[:, 2:3], AF.Ln, [fsB], [fsB], bias=EPS, scale=1.0 / D)
                ACT(stat[:, 4:5], stat[:, 3:4], AF.Exp, [fsB], [fsB], scale=-0.5)
                for half in range(2):
                    pb = 2 * j + half
                    STT(ost[:, j, half * 512:(half + 1) * 512], bank[pb], stat[:, 4:5],
                        gfin[:, half * 512:(half + 1) * 512], ALU.mult, ALU.mult, [PB[pb], fsB, gfB], [osB[j]])
            else:
                for half in range(2):
                    pb = 2 * j + half
                    CP("dve", ost[:, j, half * 512:(half + 1) * 512], bank[pb], [PB[pb]], [osB[j]])
            ob = Buf(f"out{j}")
            S.add("sp", lambda e, o=out_d[s * T + t * 128:s * T + (t + 1) * 128, :], i=ost[:, j, :]:
                  e.dma_start(out=o, in_=i), reads=[osB[j]], writes=[ob], dma_buf=ob)
            outs.append(ob)

    outs = []

    def run():
        for s in range(nseg):
            S.barrier()
            load_segment(s)
            stages = ["gla0", "mlp0", "gla1", "mlp1", "kv", "swa2", "mlp2", "swa3", "mlp3"]
            if s < nseg - 1:
                stages = stages[:5]
            for st in stages:
                if stop_after == "load":
                    break
                S.barrier()
                kind, l = st[:3], (int(st[3]) if len(st) > 3 else None)
                prefix = (s < nseg - 1)
                last = [NG - 1]
                if kind == "gla":
                    gla(l, s, out_groups=(last if (prefix and l == 1) else None))
                elif kind == "mlp":
                    mlp(l, s, preserve_kv=(l == 2), groups=(last if (prefix and l == 1) else None))
                elif st == "kv":
                    kv_phase(s, groups=(last if prefix else None))
                else:
                    swa_layer(l, s)
                if stop_after == st:
                    break
            if s == nseg - 1:
                S.barrier()
                final_store(0, with_norm=(stop_after is None))

    run()

    S.add("sp", lambda e: e.nop(), reads=outs)

    n_dma_sems = len(S.dma_bufs)
    sem_es = ExitStack()
    sems = {e: sem_es.enter_context(nc.semaphore(f"sem_{e}")) for e in ENGS}
    dma_sems = {}
    for i, b in enumerate(S.dma_bufs):
        dma_sems[id(b)] = sem_es.enter_context(nc.semaphore(f"dsem{i}"))
    print("dma semaphores:", n_dma_sems, "ops:", {e: len(S.ops[e]) for e in ENGS})
    S.prepare()
    with nc.Block() as block:
        @block.tensor
        def _(pe):
            S.emit_engine("pe", pe, sems, dma_sems)

        @block.scalar
        def _(act):
            S.emit_engine("act", act, sems, dma_sems)

        @block.vector
        def _(dve):
            S.emit_engine("dve", dve, sems, dma_sems)

        @block.gpsimd
        def _(pool):
            S.emit_engine("pool", pool, sems, dma_sems)

        @block.sync
        def _(sp):
            S.emit_engine("sp", sp, sems, dma_sems)
    sem_es.close()
    es.close()
    return nc


def _rel_bucket_band():
    BLOCK = 128
    i = np.arange(BLOCK)[:, None]
    j = np.arange(2 * BLOCK)[None, :]
    dist = i + BLOCK - j
    n = np.maximum(dist, 0)
    max_exact = 16
    large = max_exact + (np.log(np.maximum(n, 1) / max_exact) / np.log(128 / max_exact) * (32 - max_exact)).astype(np.int32)
    large = np.minimum(large, 31)
    bucket = np.where(n < max_exact, n, large).astype(np.int32)
    valid = (dist >= 0) & (dist < 128)
    return bucket, valid


def _host_consts(inp):
    f32 = np.float32
    vecs = np.zeros((128, NV), f32)

    def put(col, v):
        v = np.asarray(v, f32).reshape(-1, 128)
        for c in range(v.shape[0]):
            vecs[:, col + c] = v[c]

    for l in range(4):
        put(l * 8, inp["ln_mix"][l])
        put(32 + l * 8, inp["ln_mlp"][l])
    put(64, inp["kv_norm"])
    for l in range(2):
        put(80 + l * 4, inp["a_b_gk"][l])
        put(88 + l * 2, inp["a_onorm"][l])
    for j in range(2):
        for h in range(16):
            vecs[:, 92 + j * 16 + h] = inp["b_sinks"][j, h]
            hp, a = h // 2, h % 2
            for ti in range(2):
                vecs[:, 128 + j * 32 + hp * 4 + a * 2 + ti] = inp["b_sinks"][j, h]
    bucket, valid = _rel_bucket_band()
    biasg = np.asarray(inp["rel_table"], f32)[bucket]
    biasg = np.ascontiguousarray(biasg.transpose(0, 2, 1)).reshape(128, 16 * 256)
    maskneg = np.where(valid, 0.0, NEGM).astype(f32)
    identf = np.eye(128, dtype=f32)
    onesf = np.ones((128, 128), f32)
    p = np.arange(128)
    mask2 = ((p[:, None] // 64 == p[None, :] // 64) & (p[:, None] <= p[None, :])).astype(f32)
    scanm = np.ones((128, GS), f32)
    scanm[:, ::64] = 0.0
    gfin = np.ascontiguousarray(np.broadcast_to(np.asarray(inp["ln_final"], f32)[None, :], (128, D)))
    return dict(vecs=vecs, biasg=biasg, maskneg=maskneg, identf=identf, onesf=onesf, mask2=mask2,
                scanm=scanm, gfin=gfin)


_PROGRAM_CACHE = {}


def _get_program(nseg, stop_after=None):
    key = (nseg, stop_after)
    if key not in _PROGRAM_CACHE:
        _PROGRAM_CACHE[key] = build_program(nseg=nseg, stop_after=stop_after, dbg=stop_after is not None)
    return _PROGRAM_CACHE[key]


def kernel(x, a_w_in, a_w_gk2, a_b_gk, a_onorm, a_w_out, kv_norm, w_kv, b_w_q, b_sinks,
           b_w_out, rel_table, ln_mix, ln_mlp, w_up, w_down, ln_final):
    inp = dict(x=x, a_w_in=a_w_in, a_w_gk2=a_w_gk2, a_b_gk=a_b_gk, a_onorm=a_onorm, a_w_out=a_w_out,
               kv_norm=kv_norm, w_kv=w_kv, b_w_q=b_w_q, b_sinks=b_sinks, b_w_out=b_w_out,
               rel_table=rel_table, ln_mix=ln_mix, ln_mlp=ln_mlp, w_up=w_up, w_down=w_down, ln_final=ln_final)
    inp = {k: np.asarray(v, np.float32) for k, v in inp.items()}
    consts = _host_consts(inp)
    nc = _get_program(2)
    shared = dict(a_w_in=inp["a_w_in"], a_w_gk2=inp["a_w_gk2"], a_w_out=inp["a_w_out"], w_kv=inp["w_kv"],
                  b_w_q=inp["b_w_q"], b_w_out=inp["b_w_out"], w_up=inp["w_up"], w_down=inp["w_down"], **consts)
    ncores = 8
    in_maps = []
    for c in range(ncores):
        b, half = c // 2, c % 2
        m = dict(shared)
        xx = np.zeros((2 * T, D), np.float32)
        if half == 1:
            xx[:T] = inp["x"][b, :T]
        xx[T:] = inp["x"][b, half * T:(half + 1) * T]
        m["x"] = xx
        v = consts["vecs"].copy()
        v[:, 124] = NEGM if half == 0 else 0.0
        m["vecs"] = v
        in_maps.append(m)
    res = run_bass_kernel_spmd(nc, in_maps, core_ids=list(range(ncores)))
    out = np.empty((4, SEQ, D), np.float32)
    for c in range(ncores):
        b, half = c // 2, c % 2
        out[b, half * T:(half + 1) * T] = np.asarray(res.results[c]["out"], np.float32)
    return out
```

```python
import numpy as np
import concourse.bass as bass
import concourse.mybir as mybir
from concourse.bass_utils import run_bass_kernel_spmd

F32 = mybir.dt.float32
BF16 = mybir.dt.bfloat16
AF = mybir.ActivationFunctionType
ALU = mybir.AluOpType
AX = mybir.AxisListType

D = 1024
SEQ = 4096
T = 2048
NG = 4
GS = 512
NT = 16
DFF = 4096
EPS = 1e-6
NEGM = -30000.0
NV = 192
DBG_SWA_LEVEL = 99

class Buf:
    __slots__ = ("name", "w", "r", "sem", "cnt")

    def __init__(self, name):
        self.name = name
        self.w = None
        self.r = {}
        self.sem = None
        self.cnt = 0


class Op:
    __slots__ = ("eng", "fn", "deps", "pos", "inc_ok", "needs_inc", "tok", "dma", "dsem", "dcnt")


ENGS = ("pe", "act", "dve", "pool", "sp")


class Sched:
    def __init__(self):
        self.ops = {e: [] for e in ENGS}
        self.extra = {e: set() for e in ENGS}
        self.dma_bufs = []
        self.all_dma = []
        self.semreg = {}

    def add(self, eng, fn, reads=(), writes=(), inc_ok=True, dma_buf=None, skip_waw=False):
        op = Op()
        op.eng = eng
        op.fn = fn
        op.pos = len(self.ops[eng])
        op.inc_ok = True
        op.needs_inc = False
        op.tok = None
        op.dma = dma_buf is not None
        deps = set(self.extra[eng])
        self.extra[eng] = set()
        for b in reads:
            if b.w is not None:
                deps.add(b.w)
        for b in writes:
            if b.w is not None and not (skip_waw and b is dma_buf):
                deps.add(b.w)
            for r in b.r.values():
                deps.add(r)
        deps.discard(op)
        op.deps = deps
        if op.dma:
            sh = self.semreg.get(dma_buf.name)
            if sh is None:
                sh = Buf("sem:" + dma_buf.name)
                self.semreg[dma_buf.name] = sh
                self.dma_bufs.append(sh)
            sh.cnt += 16
            sh.w = op
            op.dsem = sh
            op.dcnt = sh.cnt
            self.all_dma.append(op)
        for b in writes:
            b.w = op
            b.r = {}
        wset = set(id(b) for b in writes)
        for b in reads:
            if id(b) in wset:
                continue
            key = ("dma", id(op)) if op.dma else eng
            b.r[key] = op
        self.ops[eng].append(op)
        return op

    def barrier(self):
        last = set()
        for e in ENGS:
            if self.ops[e]:
                last.add(self.ops[e][-1])
        for b in self.dma_bufs:
            if b.w is not None and b.w.dma:
                last.add(b.w)
        for op in self.all_dma[-64:]:
            last.add(op)
        for e in ENGS:
            self.extra[e] |= last

    def prepare(self):
        for e in ENGS:
            ops = self.ops[e]
            for op in reversed(ops):
                if not op.dma:
                    op.inc_ok = True
                    break
        nxt = {}
        for e in ENGS:
            ops = self.ops[e]
            arr = [None] * len(ops)
            cur = None
            for i in range(len(ops) - 1, -1, -1):
                if (not ops[i].dma) and ops[i].inc_ok:
                    cur = ops[i]
                arr[i] = cur
            nxt[e] = arr

        def resolve(x):
            if x.dma:
                return x
            r = nxt[x.eng][x.pos]
            assert r is not None, (x.eng, x.pos)
            return r

        for e in ENGS:
            for y in self.ops[e]:
                nd = set()
                for x in y.deps:
                    if (not x.dma) and x.eng == e:
                        if e == "pe":
                            continue
                    rx = resolve(x)
                    if not rx.dma:
                        rx.needs_inc = True
                    nd.add(rx)
                y.deps = nd
        for e in ENGS:
            n = 0
            for op in self.ops[e]:
                if (not op.dma) and op.needs_inc:
                    n += 1
                    op.tok = n

    def emit_engine(self, e, eng, sems, dma_sems):
        known = {}
        nwaits = 0
        for y in self.ops[e]:
            waits = {}
            for x in y.deps:
                if x.dma:
                    k = ("d", id(x.dsem))
                    s = dma_sems[id(x.dsem)]
                    v = x.dcnt
                else:
                    k = ("e", x.eng)
                    s = sems[x.eng]
                    v = x.tok
                if known.get(k, 0) >= v:
                    continue
                if k not in waits or waits[k][1] < v:
                    waits[k] = (s, v)
            for k, (s, v) in waits.items():
                eng.wait_ge(s, v)
                known[k] = v
                nwaits += 1
            ins = y.fn(eng)
            if y.dma:
                ins.then_inc(dma_sems[id(y.dsem)], 16)
            elif y.needs_inc:
                ins.then_inc(sems[e], 1)
        return nwaits


def build_program(nseg=2, stop_after=None, dbg=False):
    nc = bass.Bass("TRN2", target_bir_lowering=False)
    S = Sched()
    ntok = nseg * T

    def din(name, shape):
        return nc.dram_tensor(name, list(shape), F32, kind="ExternalInput").ap()

    x_d = din("x", [ntok, D])
    a_w_in = din("a_w_in", [2, D, 3088])
    a_w_gk2 = din("a_w_gk2", [2, 16, 512])
    a_w_out = din("a_w_out", [2, D, D])
    w_kv = din("w_kv", [D, 256])
    b_w_q = din("b_w_q", [2, D, D])
    b_w_out = din("b_w_out", [2, D, D])
    w_up = din("w_up", [4, D, DFF])
    w_down = din("w_down", [4, DFF, D])
    vecs_d = din("vecs", [128, NV])
    biasg_d = din("biasg", [128, 16 * 256])
    maskneg_d = din("maskneg", [128, 256])
    identf_d = din("identf", [128, 128])
    ones_d = din("onesf", [128, 128])
    mask2_d = din("mask2", [128, 128])
    scanm_d = din("scanm", [128, GS])
    gfin_d = din("gfin", [128, D])
    out_d = nc.dram_tensor("out", [T, D], F32, kind="ExternalOutput").ap()

    from contextlib import ExitStack
    es = ExitStack()

    def sb(name, shape, dt):
        return es.enter_context(nc.sbuf_tensor("s_" + name, list(shape), dt))

    hT = sb("hT", [128, 8, T], F32)
    AT = sb("AT", [128, 8, T], BF16)
    UBN = 36480
    UFN = 6720
    UB = sb("UB", [128, UBN], BF16)
    UF = sb("UF", [128, UFN], F32)
    vecs = sb("vecs", [128, NV], F32)
    nvec = sb("nvec", [128, 104], F32)
    identf = sb("identf", [128, 128], F32)
    cb = sb("cb", [128, 768], BF16)
    mask2 = sb("mask2", [128, 128], F32)
    Rst = sb("Rst", [128, 9, 256], F32)
    elast = sb("elast", [128, 8], F32)
    haloK = sb("haloK", [128, 2, 128], BF16)
    haloV = sb("haloV", [128, 2, 192], BF16)
    print("sbuf bytes remaining:", nc.sbuf_bytes_remaining)

    PS = [es.enter_context(nc.psum_tensor(f"ps{i}", [128, 512], F32)) for i in range(7)]
    PT = es.enter_context(nc.psum_tensor("pst", [128, 1024], BF16))
    PS.append(PT[:, :].bitcast(F32))
    bank = [p[:, :] if not isinstance(p, bass.AP) else p for p in PS]

    hB = [[Buf(f"h{c}_{g}") for g in range(NG)] for c in range(8)]
    AB = [[Buf(f"A{c}_{g}") for g in range(NG)] for c in range(8)]
    PB = [Buf(f"bank{i}") for i in range(8)]
    constB = Buf("consts")
    identb = cb[:, 0:128]
    onesb = cb[:, 128:256]

    def c2d(ap):
        return ap

    def MM(out, lhsT, rhs, start, stop, reads, writes, inc=None):
        inc_ok = stop if inc is None else inc
        return S.add("pe", lambda e: e.matmul(out, lhsT, rhs, start=start, stop=stop),
                     reads=reads, writes=writes, inc_ok=inc_ok)

    def TR(out, in_, ident, reads, writes, inc=True):
        return S.add("pe", lambda e: e.transpose(out, in_, ident), reads=reads, writes=writes, inc_ok=inc)

    def ACT(out, in_, func, reads, writes, bias=None, scale=None, accum=None):
        kw = {}
        if bias is not None:
            kw["bias"] = bias
        if scale is not None:
            kw["scale"] = scale
        if accum is not None:
            kw["accum_out"] = accum
        return S.add("act", lambda e: e.activation(out, in_, func, **kw), reads=reads, writes=writes)

    def TT(eng, out, in0, in1, op, reads, writes):
        return S.add(eng, lambda e: e.tensor_tensor(out, in0, in1, op), reads=reads, writes=writes)

    def TS(eng, out, in0, s1, op0, reads, writes, s2=None, op1=None):
        if op1 is None:
            return S.add(eng, lambda e: e.tensor_scalar(out, in0, s1, None, op0), reads=reads, writes=writes)
        return S.add(eng, lambda e: e.tensor_scalar(out, in0, s1, s2, op0, op1), reads=reads, writes=writes)

    def STT(out, in0, scalar, in1, op0, op1, reads, writes):
        return S.add("dve", lambda e: e.scalar_tensor_tensor(out, in0, scalar, in1, op0, op1),
                     reads=reads, writes=writes)

    def CP(eng, out, in_, reads, writes):
        if eng == "act":
            return S.add("act", lambda e: e.copy(out, in_), reads=reads, writes=writes)
        return S.add(eng, lambda e: e.tensor_copy(out, in_), reads=reads, writes=writes)

    def DMA(q, out, in_, wbuf, reads=(), skip_waw=False, extra_writes=()):
        return S.add(q, lambda e: e.dma_start(out=out, in_=in_), reads=reads,
                     writes=(wbuf,) + tuple(extra_writes), dma_buf=wbuf, skip_waw=skip_waw)

    def MEMSET(eng, ap, val, writes):
        return S.add(eng, lambda e: e.memset(ap, val), writes=writes)

    DMA("sp", vecs[:, :], vecs_d[:, :], constB)
    DMA("sp", identf[:, :], identf_d[:, :], constB, skip_waw=True)
    DMA("sp", mask2[:, :], mask2_d[:, :], constB, skip_waw=True)
    cbB = Buf("cb")
    DMA("pool", cb[:, 0:128], identf_d[:, :], cbB)
    DMA("pool", cb[:, 128:256], ones_d[:, :], cbB, skip_waw=True)
    DMA("pool", cb[:, 256:768], scanm_d[:, :], cbB, skip_waw=True)
    scanm = cb[:, 256:768]
    nvB = Buf("nvec")
    TS("dve", nvec[:, 0:8], vecs[:, 80:88], -1.0, ALU.mult, [constB], [nvB])
    TS("dve", nvec[:, 8:40], vecs[:, 92:124], -1.0, ALU.mult, [constB], [nvB])
    TS("dve", nvec[:, 40:104], vecs[:, 128:192], -1.0, ALU.mult, [constB], [nvB])
    stB = Buf("state")
    MEMSET("dve", Rst[:, :, :], 0.0, [stB])
    MEMSET("dve", elast[:, :], 1.0, [stB])
    hkB = Buf("haloK")
    hvB = Buf("haloV")
    MEMSET("dve", haloK[:, :, :], 0.0, [hkB])
    MEMSET("dve", haloV[:, :, :], 0.0, [hvB])

    def gcol(i):
        return vecs[:, i:i + 1]

    class Carver:
        def __init__(self, t, n):
            self.t = t
            self.n = n
            self.off = 0

        def take(self, n, shape=None):
            a = self.t[:, self.off:self.off + n]
            self.off += n
            assert self.off <= self.n, (self.off, self.n)
            if shape is not None:
                if len(shape) == 2:
                    a = a.rearrange("p (a b) -> p a b", a=shape[0], b=shape[1])
                elif len(shape) == 3:
                    a = a.rearrange("p (a b c) -> p a b c", a=shape[0], b=shape[1], c=shape[2])
            return a

    def rmsnorm(cf, cbf, gbase, groups=None):
        nsq = cbf.take(2 * GS, (2, GS))
        nln = cf.take(GS)
        nrs = cf.take(2 * GS, (2, GS))
        sqB = [Buf("nsq0"), Buf("nsq1")]
        lnB = Buf("nln")
        rsB = [Buf("nrs0"), Buf("nrs1")]
        for g in (range(NG) if groups is None else groups):
            pb = 0
            for c in range(8):
                j = c % 2
                ACT(nsq[:, j, :], hT[:, c, g * GS:(g + 1) * GS], AF.Square, [hB[c][g]], [sqB[j]])
                MM(bank[pb], onesb, nsq[:, j, :], c == 0, c == 7, [sqB[j], cbB], [PB[pb]])
            ACT(nln[:, :], bank[pb], AF.Ln, [PB[pb]], [lnB], bias=EPS, scale=1.0 / D)
            r = g % 2
            ACT(nrs[:, r, :], nln[:, :], AF.Exp, [lnB], [rsB[r]], scale=-0.5)
            for c in range(8):
                STT(AT[:, c, g * GS:(g + 1) * GS], hT[:, c, g * GS:(g + 1) * GS], gcol(gbase + c),
                    nrs[:, r, :], ALU.mult, ALU.mult, [hB[c][g], rsB[r], constB], [AB[c][g]])

    def load_segment(s):
        cf = Carver(UF, UFN)
        xin = cf.take(2 * D, (2, D))
        xB = [Buf("xin0"), Buf("xin1")]
        for t in range(NT):
            j = t % 2
            g = t // 4
            DMA("sp", xin[:, j, :], x_d[s * T + t * 128: s * T + (t + 1) * 128, :], xB[j])
            for half in range(2):
                pb = 2 * j + half
                for cc in range(4):
                    c = half * 4 + cc
                    TR(bank[pb][:, cc * 128:(cc + 1) * 128], xin[:, j, c * 128:(c + 1) * 128], identf[:, :],
                       [xB[j], constB], [PB[pb]], inc=(cc == 3))
                dst = hT[:, half * 4:half * 4 + 4, t * 128:(t + 1) * 128]
                src = bank[pb].rearrange("p (a b) -> p a b", a=4, b=128)
                eng = "act" if half == 0 else "dve"
                CP(eng, dst, src, [PB[pb]], [hB[half * 4 + cc][g] for cc in range(4)])

    def wload(dst, src, wbuf, first=True):
        DMA("pool", dst, src, wbuf, skip_waw=not first)

    def rows_view(w2d):
        return w2d.rearrange("(kc p) f -> p kc f", p=128)

    def mlp(l, s, preserve_kv=False, groups=None):
        groups = list(range(NG)) if groups is None else list(groups)
        cf = Carver(UF, UF_LO if preserve_kv else UFN)
        cbf = Carver(UB, UB_LO if preserve_kv else UBN)
        rmsnorm(cf, cbf, 32 + l * 8, groups)
        rt = cf.take(2 * GS, (2, GS))
        rtB = [Buf("rt0"), Buf("rt1")]
        u2 = cbf.take(4 * T, (4, T))
        u2B = [[Buf(f"u2_{c}_{g}") for g in range(NG)] for c in range(4)]
        wu = [cbf.take(8 * 512, (8, 512)) for _ in range(2)]
        wd = [cbf.take(4 * 1024, (4, 1024)) for _ in range(2)]
        wuB = [Buf("wu0"), Buf("wu1")]
        wdB = [Buf("wd0"), Buf("wd1")]
        wupv = rows_view(w_up[l])
        wdnv = rows_view(w_down[l])
        NF = DFF // 512

        def load(f):
            j = f % 2
            wload(wu[j], wupv[:, :, f * 512:(f + 1) * 512], wuB[j])
            wload(wd[j], wdnv[:, f * 4:(f + 1) * 4, :], wdB[j])

        load(0)
        setc = 0
        ri = 0
        for f in range(NF):
            j = f % 2
            if f + 1 < NF:
                load(f + 1)
            for fc in range(4):
                bs = (setc % 2) * 4
                setc += 1
                for kc in range(8):
                    for g in groups:
                        MM(bank[bs + g], wu[j][:, kc, fc * 128:(fc + 1) * 128], AT[:, kc, g * GS:(g + 1) * GS],
                           kc == 0, kc == 7, [wuB[j], AB[kc][g]], [PB[bs + g]])
                for g in groups:
                    r = ri % 2
                    ri += 1
                    ACT(rt[:, r, :], bank[bs + g], AF.Relu, [PB[bs + g]], [rtB[r]])
                    TT("dve", u2[:, fc, g * GS:(g + 1) * GS], rt[:, r, :], rt[:, r, :], ALU.mult,
                       [rtB[r]], [u2B[fc][g]])
            for oc in range(8):
                bs = (setc % 2) * 4
                setc += 1
                for k2 in range(4):
                    for g in groups:
                        MM(bank[bs + g], wd[j][:, k2, oc * 128:(oc + 1) * 128], u2[:, k2, g * GS:(g + 1) * GS],
                           k2 == 0, k2 == 3, [wdB[j], u2B[k2][g]], [PB[bs + g]])
                for g in groups:
                    hv = hT[:, oc, g * GS:(g + 1) * GS]
                    TT("dve", hv, bank[bs + g], hv, ALU.add, [PB[bs + g], hB[oc][g]], [hB[oc][g]])

    def gla(l, s, out_groups=None):
        out_groups = set(range(NG)) if out_groups is None else set(out_groups)
        cf = Carver(UF, UFN)
        cbf = Carver(UB, UBN)
        rmsnorm(cf, cbf, l * 8)
        tE = cf.take(GS)
        tL = cf.take(GS)
        Lc = cf.take(GS)
        Ae = [cf.take(GS) for _ in range(2)]
        Ai = cf.take(GS)
        rstd = cf.take(GS)
        lnv = cf.take(GS)
        sgr = cf.take(2 * GS, (2, GS))
        tEB, tLB, LcB, AiB, rsB, lnB = (Buf(n) for n in ("tE", "tL", "Lc", "Ai", "grs", "gln"))
        AeB = [Buf("Ae0"), Buf("Ae1")]
        sgrB = [Buf("sgr0"), Buf("sgr1")]
        glrT = cbf.take(T)
        wgk = cbf.take(512)
        wglr = cbf.take(8 * 16, (8, 16))
        qt = [cbf.take(GS) for _ in range(2)]
        kt = [cbf.take(GS) for _ in range(2)]
        vtok = [cbf.take(4 * 256, (4, 256)) for _ in range(2)]
        ktok = [cbf.take(4 * 128, (4, 128)) for _ in range(2)]
        sg = [cbf.take(2 * GS, (2, GS)) for _ in range(2)]
        attm = cbf.take(2 * 128, (2, 128))
        Sbf = cbf.take(8 * 256, (8, 256))
        sq = cbf.take(2 * GS, (2, GS))
        og = [cbf.take(2 * GS, (2, GS)) for _ in range(2)]
        wqkv = [cbf.take(8 * 512, (8, 512)) for _ in range(2)]
        wg = [cbf.take(8 * 256, (8, 256)) for _ in range(2)]
        wo = [cbf.take(2 * 1024, (2, 1024)) for _ in range(2)]
        glrB, wgkB, wglrB = (Buf(n) for n in ("glrT", "wgk", "wglr"))
        qtB = [Buf("qt0"), Buf("qt1")]
        ktB = [Buf("kt0"), Buf("kt1")]
        vtB = [Buf("vtok0"), Buf("vtok1")]
        ktkB = [Buf("ktok0"), Buf("ktok1")]
        sgB = [[Buf(f"sg{p}_{v}") for v in range(2)] for p in range(2)]
        atB = [Buf("att0"), Buf("att1")]
        SbB = [Buf(f"Sbf{c}") for c in range(8)]
        sqB = [Buf("gsq0"), Buf("gsq1")]
        ogB = [[Buf(f"og{p}_{v}") for v in range(2)] for p in range(2)]
        wqkvB = [Buf("wqkv0"), Buf("wqkv1")]
        wgB = [Buf("wg0"), Buf("wg1")]
        woB = [Buf("wo0"), Buf("wo1")]
        RB = [[Buf(f"R{h}_{p}") for p in range(2)] for h in range(4)]
        elB = [Buf(f"el{h}") for h in range(4)]
        for h in range(4):
            for p in range(2):
                RB[h][p].w = stB.w
            elB[h].w = stB.w

        win = rows_view(a_w_in[l])
        wov = rows_view(a_w_out[l])

        wload(wgk[0:16, :], a_w_gk2[l], wgkB)
        wload(wglr, win[:, :, 3072:3088], wglrB)

        def load_head(h):
            j = h % 2
            wload(wqkv[j][:, :, 0:128], win[:, :, h * 128:(h + 1) * 128], wqkvB[j])
            wload(wqkv[j][:, :, 128:256], win[:, :, 512 + h * 128:512 + (h + 1) * 128], wqkvB[j], first=False)
            wload(wqkv[j][:, :, 256:512], win[:, :, 1024 + h * 256:1024 + (h + 1) * 256], wqkvB[j], first=False)
            wload(wg[j], win[:, :, 2048 + h * 256:2048 + (h + 1) * 256], wgB[j])

        def load_wo(h):
            wload(wo[h % 2], wov[:, 2 * h:2 * h + 2, :], woB[h % 2])

        load_head(0)
        load_head(1)
        load_wo(0)
        load_wo(1)
        for g in range(NG):
            for kc in range(8):
                MM(bank[g][0:16, :], wglr[:, kc, :], AT[:, kc, g * GS:(g + 1) * GS], kc == 0, kc == 7,
                   [wglrB, AB[kc][g]], [PB[g]])
            CP("act", glrT[0:16, g * GS:(g + 1) * GS], bank[g][0:16, :], [PB[g]], [glrB])

        units = [(h, g) for h in range(4) for g in range(NG)]
        NU = len(units)
        rp = [0, 0, 0, 0]
        oset = [0]

        def G0(u):
            h, g = units[u]
            p = u % 2
            st = l * 4 + h
            gs = slice(g * GS, (g + 1) * GS)
            MM(bank[0], wgk[0:16, h * 128:(h + 1) * 128], glrT[0:16, gs], True, True, [wgkB, glrB], [PB[0]])
            ACT(tE, bank[0], AF.Exp, [PB[0], nvB], [tEB], bias=nvec[:, st:st + 1], scale=-1.0)
            ACT(tL, tE, AF.Ln, [tEB], [tLB], bias=1.0)
            S.add("dve", lambda e, o=Lc, a_=scanm, b_=tL: e.tensor_tensor_scan(o, a_, b_, 0.0, ALU.mult, ALU.add),
                  reads=[tLB, cbB], writes=[LcB])
            ACT(Ae[p], Lc, AF.Exp, [LcB], [AeB[p]], scale=-1.0 / 16.0)
            ACT(Ai, Lc, AF.Exp, [LcB], [AiB], scale=1.0 / 16.0)

        def G1(u):
            h, g = units[u]
            p = u % 2
            j = h % 2
            gs = slice(g * GS, (g + 1) * GS)
            if g == 0 and h >= 1 and h + 1 < 4:
                load_head(h + 1)
            need_out = g in out_groups
            if need_out:
                for kc in range(8):
                    MM(bank[1], wqkv[j][:, kc, 0:128], AT[:, kc, gs], kc == 0, kc == 7, [wqkvB[j], AB[kc][g]], [PB[1]])
                STT(qt[p], bank[1], float(128 ** -0.5), Ae[p], ALU.mult, ALU.mult, [PB[1], AeB[p]], [qtB[p]])
                yield
            for kc in range(8):
                MM(bank[0], wqkv[j][:, kc, 128:256], AT[:, kc, gs], kc == 0, kc == 7, [wqkvB[j], AB[kc][g]], [PB[0]])
            TT("dve", kt[p], bank[0], Ai, ALU.mult, [PB[0], AiB], [ktB[p]])
            yield
            for half in range(2):
                pb = 1 - half
                for t2 in range(2):
                    tt = half * 2 + t2
                    for kc in range(8):
                        MM(bank[pb][:, t2 * 256:(t2 + 1) * 256],
                           AT[:, kc, g * GS + tt * 128:g * GS + (tt + 1) * 128], wqkv[j][:, kc, 256:512],
                           kc == 0, kc == 7, [wqkvB[j], AB[kc][g]], [PB[pb]])
                CP("act", vtok[p][:, 2 * half:2 * half + 2, :],
                   bank[pb].rearrange("p (a b) -> p a b", a=2, b=256), [PB[pb]], [vtB[p]])
                yield
            for vc in range(2):
                if not need_out:
                    break
                pb = 1 - vc
                for kc in range(8):
                    MM(bank[pb], wg[j][:, kc, vc * 128:(vc + 1) * 128], AT[:, kc, gs], kc == 0, kc == 7,
                       [wgB[j], AB[kc][g]], [PB[pb]])
                ACT(sg[p][:, vc, :], bank[pb], AF.Silu, [PB[pb]], [sgB[p][vc]])
                yield
            b1h = bank[1].bitcast(BF16)
            for tt in range(4):
                TR(b1h[:, tt * 128:(tt + 1) * 128], kt[p][:, tt * 128:(tt + 1) * 128], identb, [ktB[p], cbB], [PB[1]])
            CP("dve", ktok[p], b1h[:, 0:512].rearrange("p (a b) -> p a b", a=4, b=128), [PB[1]], [ktkB[p]])

        def G2(u):
            h, g = units[u]
            p = u % 2
            st = l * 4 + h
            R = [Rst[:, st, :], Rst[:, 8, :]]
            el = elast[:, st:st + 1]
            for c in range(8):
                tt = c // 2
                r0 = (c % 2) * 64
                pb = 2 + (c % 2)
                MM(bank[pb][:, 0:256], ktok[p][r0:r0 + 64, tt, :], vtok[p][r0:r0 + 64, tt, :], True, True,
                   [ktkB[p], vtB[p]], [PB[pb]])
                eprev = el if c == 0 else Ae[p][:, c * 64 - 1:c * 64]
                erd = [elB[h]] if c == 0 else [AeB[p]]
                r_ = rp[h]
                if g in out_groups:
                    TS("dve", Sbf[:, c, :], R[r_], eprev, ALU.mult, [RB[h][r_]] + erd, [SbB[c]])
                STT(R[1 - r_], R[r_], eprev, bank[pb][:, 0:256], ALU.mult, ALU.add,
                    [RB[h][r_], PB[pb]] + erd, [RB[h][1 - r_]])
                rp[h] = 1 - r_
                yield
            CP("dve", el, Ae[p][:, GS - 1:GS], [AeB[p]], [elB[h]])
            for tt in range(4):
                if g not in out_groups:
                    break
                ts_ = slice(tt * 128, (tt + 1) * 128)
                MM(bank[4][:, 0:128], kt[p][:, ts_], qt[p][:, ts_], True, True, [ktB[p], qtB[p]], [PB[4]])
                TT("dve", attm[:, tt % 2, :], bank[4][:, 0:128], mask2[:, :], ALU.mult, [PB[4], constB], [atB[tt % 2]])
                for vc in range(2):
                    po = 5 + vc
                    MM(bank[po][:, ts_], vtok[p][:, tt, vc * 128:(vc + 1) * 128], attm[:, tt % 2, :], True, False,
                       [vtB[p], atB[tt % 2]], [PB[po]])
                    for hf in range(2):
                        c = 2 * tt + hf
                        cs = slice(tt * 128 + hf * 64, tt * 128 + hf * 64 + 64)
                        MM(bank[po][:, cs], Sbf[:, c, vc * 128:(vc + 1) * 128], qt[p][:, cs], False, hf == 1,
                           [SbB[c], qtB[p]], [PB[po]])
                yield

        def G3(u):
            h, g = units[u]
            if g not in out_groups:
                return
            p = u % 2
            for vc in range(2):
                ACT(sq[:, vc, :], bank[5 + vc], AF.Square, [PB[5 + vc]], [sqB[vc]])
                MM(bank[2], onesb, sq[:, vc, :], vc == 0, vc == 1, [sqB[vc], cbB], [PB[2]])
            ACT(lnv, bank[2], AF.Ln, [PB[2]], [lnB], bias=EPS, scale=1.0 / 256.0)
            ACT(rstd, lnv, AF.Exp, [lnB], [rsB], scale=-0.5)
            for vc in range(2):
                TT("dve", sgr[:, vc, :], sg[p][:, vc, :], rstd, ALU.mult, [sgB[p][vc], rsB], [sgrB[vc]])
                STT(og[p][:, vc, :], bank[5 + vc], gcol(88 + l * 2 + vc), sgr[:, vc, :], ALU.mult, ALU.mult,
                    [PB[5 + vc], sgrB[vc], constB], [ogB[p][vc]])

        def G4(u):
            h, g = units[u]
            p = u % 2
            j = h % 2
            for oc in range(8):
                if g not in out_groups:
                    break
                pb = 7 if oset[0] % 2 == 0 else 4
                oset[0] += 1
                for k2 in range(2):
                    MM(bank[pb], wo[j][:, k2, oc * 128:(oc + 1) * 128], og[p][:, k2, :], k2 == 0, k2 == 1,
                       [woB[j], ogB[p][k2]], [PB[pb]])
                hv = hT[:, oc, g * GS:(g + 1) * GS]
                TT("dve", hv, bank[pb], hv, ALU.add, [PB[pb], hB[oc][g]], [hB[oc][g]])
                yield
            if g == NG - 1 and h + 2 < 4:
                load_wo(h + 2)

        def drain(gen):
            for _ in gen:
                pass

        for k in range(NU + 5):
            if 0 <= k - 3 < NU:
                G3(k - 3)
            g4 = G4(k - 4) if 0 <= k - 4 < NU else iter(())
            g2 = G2(k - 2) if 0 <= k - 2 < NU else iter(())
            g1 = G1(k - 1) if 0 <= k - 1 < NU else iter(())
            alive = True
            while alive:
                alive = False
                for gen in (g2, g1, g4):
                    try:
                        next(gen)
                        alive = True
                    except StopIteration:
                        pass
            if 0 <= k < NU:
                G0(k)

    KV_ELEMS = 2 * 17 * 128 + 17 * 2 * 192
    UB_LO = UBN - KV_ELEMS
    UF_LO = UFN - 16 * 256
    swa_ctx = {}

    def kv_phase(s, groups=None):
        groups = list(range(NG)) if groups is None else list(groups)
        cf = Carver(UF, UF_LO)
        cbf = Carver(UB, UB_LO)
        rmsnorm(cf, cbf, 64, groups)
        top = Carver(UB, UBN)
        top.off = UB_LO
        KT = top.take(2 * 17 * 128, (2, 17 * 128))
        Vp = top.take(17 * 2 * 192, (17, 2, 192))
        ftop = Carver(UF, UFN)
        ftop.off = UF_LO
        biasT = ftop.take(16 * 256, (16, 256))
        biasB = Buf("biasT")
        KTB = [Buf("KT_halo")] + [Buf(f"KT{g}") for g in range(NG)]
        VpB = [Buf("Vp_halo")] + [Buf(f"Vp{g}") for g in range(NG)]
        swa_ctx.update(KT=KT, Vp=Vp, biasT=biasT, biasB=biasB, KTB=KTB, VpB=VpB)
        mk = cf.take(256)
        mkB = Buf("mk")
        DMA("sp", biasT.rearrange("p a b -> p (a b)"), biasg_d[:, :], biasB)
        DMA("sp", mk, maskneg_d[:, :], mkB)
        for hh in range(16):
            TT("dve", biasT[:, hh, :], biasT[:, hh, :], mk, ALU.add, [biasB, mkB], [biasB])
        wkd = cbf.take(8 * 256, (8, 2, 128))
        wvv = cbf.take(8 * 128, (8, 128))
        wkB, wvB = Buf("wkd"), Buf("wvv")
        wkvv = rows_view(w_kv)
        first = True
        for gk in range(2):
            for d2 in range(2):
                wload(wkd[:, :, gk, d2 * 64:(d2 + 1) * 64], wkvv[:, :, gk * 64:(gk + 1) * 64], wkB, first=first)
                first = False
        wload(wvv, wkvv[:, :, 128:256], wvB)
        MEMSET("pool", Vp[:, :, :, :], 0.0, VpB)
        CP("pool", KT[:, :, 0:128], haloK[:, :, :], [hkB], [KTB[0]])
        CP("pool", Vp[:, 0, :, :], haloV[:, :, :], [hvB], [VpB[0]])
        for gk in range(2):
            for kc in range(8):
                for g in groups:
                    MM(bank[g], wkd[:, kc, gk, :], AT[:, kc, g * GS:(g + 1) * GS], kc == 0, kc == 7,
                       [wkB, AB[kc][g]], [PB[g]])
            for g in groups:
                CP("act", KT[:, gk, 128 + g * GS:128 + (g + 1) * GS], bank[g], [PB[g]], [KTB[1 + g]])
        for g in groups:
            pb = 4 + (g % 2)
            for tt in range(4):
                for kc in range(8):
                    MM(bank[pb][:, tt * 128:(tt + 1) * 128], AT[:, kc, g * GS + tt * 128:g * GS + (tt + 1) * 128],
                       wvv[:, kc, :], kc == 0, kc == 7, [wvB, AB[kc][g]], [PB[pb]], inc=(kc == 7 and tt == 3))
            CP("act", Vp[:, 1 + 4 * g:5 + 4 * g, :, 64:128],
               bank[pb].rearrange("p (a b c) -> p a b c", a=4, b=2, c=64), [PB[pb]], [VpB[1 + g]])
        CP("pool", haloK[:, :, :], KT[:, :, 16 * 128:17 * 128], [KTB[NG]], [hkB])
        CP("pool", haloV[:, :, :], Vp[:, 16, :, :], [VpB[NG]], [hvB])

    def swa_layer(l, s):
        jj = l - 2
        KT, Vp, biasT, biasB = swa_ctx["KT"], swa_ctx["Vp"], swa_ctx["biasT"], swa_ctx["biasB"]
        KTB, VpB = swa_ctx["KTB"], swa_ctx["VpB"]
        cf = Carver(UF, UF_LO)
        cbf = Carver(UB, UB_LO)
        rmsnorm(cf, cbf, l * 8)
        S.barrier()
        cf = Carver(UF, UF_LO)
        cbf = Carver(UB, UB_LO)
        ssb = [cf.take(1024, (2, 2, 256)) for _ in range(2)]
        stat = [cf.take(32) for _ in range(4)]
        Wq = cbf.take(8 * 1024, (8, 1024))
        Wo = cbf.take(8 * 1024, (8, 1024))
        qh = cbf.take(2 * GS, (2, GS))
        oTg = cbf.take(8 * GS, (8, GS))
        pbuf = [cbf.take(1024, (2, 2, 256)) for _ in range(2)]
        dg = [cbf.take(512, (4, 128)) for _ in range(2)]
        pnT = cbf.take(1024)
        ssB = [Buf("ss0"), Buf("ss1")]
        WqB, WoB = Buf("Wq"), Buf("Wo")
        qhB = [Buf("qh0"), Buf("qh1")]
        oTB = [Buf(f"oT{c}") for c in range(8)]
        pB = [Buf("p0"), Buf("p1")]
        pnB = Buf("pn")
        dgB = [Buf("dg0"), Buf("dg1")]
        stB_ = [Buf(f"stat{i}") for i in range(4)]
        for hf in range(2):
            wload(Wq[:, :, hf * 512:(hf + 1) * 512], rows_view(b_w_q[jj])[:, :, hf * 512:(hf + 1) * 512], WqB, first=(hf == 0))
        for hf in range(2):
            wload(Wo[:, :, hf * 512:(hf + 1) * 512], rows_view(b_w_out[jj])[:, :, hf * 512:(hf + 1) * 512], WoB, first=(hf == 0))
        units = [(g, hp, su) for g in range(NG) for hp in range(8) for su in range(2)]
        NU = len(units)
        BQ, BO = 7, 6

        def ktb(t):
            r = [KTB[1 + (t // 4)]]
            r.append(KTB[0] if t == 0 else KTB[1 + ((t - 1) // 4)])
            return r

        def vpb(i):
            return VpB[0] if i == 0 else VpB[1 + (i - 1) // 4]

        def st_q(u):
            g, hp, su = units[u]
            if su != 0:
                return
            qb = hp % 2
            for kc in range(8):
                MM(bank[BQ], Wq[:, kc, hp * 128:(hp + 1) * 128], AT[:, kc, g * GS:(g + 1) * GS],
                   kc == 0, kc == 7, [WqB, AB[kc][g]], [PB[BQ]])
            S.add("act", lambda e, o=qh[:, qb, :], i=bank[BQ]: e.mul(o, i, 0.125), reads=[PB[BQ]], writes=[qhB[qb]])

        def st0(u):
            g, hp, su = units[u]
            x = u % 2
            gk = hp // 4
            qb = hp % 2
            for ti in range(2):
                tt = 2 * su + ti
                t = g * 4 + tt
                for a in range(2):
                    r0 = a * 64
                    MM(bank[2 * x + a][:, ti * 256:(ti + 1) * 256], qh[r0:r0 + 64, qb, tt * 128:(tt + 1) * 128],
                       KT[r0:r0 + 64, gk, t * 128:(t + 2) * 128], True, True, [qhB[qb]] + ktb(t), [PB[2 * x + a]])

        def st1(u):
            g, hp, su = units[u]
            x = u % 2
            for a in range(2):
                for ti in range(2):
                    TT("dve", ssb[x][:, a, ti, :], bank[2 * x + a][:, ti * 256:(ti + 1) * 256],
                       biasT[:, 2 * hp + a, :], ALU.add, [PB[2 * x + a], biasB], [ssB[x]])
            if g == 0 and su == 0:
                for a in range(2):
                    TS("dve", ssb[x][:, a, 0, 0:128], ssb[x][:, a, 0, 0:128], vecs[:, 124:125], ALU.add,
                       [ssB[x], constB], [ssB[x]])
            y = u % 4
            st_ = stat[y]
            S.add("dve", lambda e, o=st_[:, 0:4], i=ssb[x].rearrange("p a t k -> p (a t) k"):
                  e.tensor_reduce(o, i, AX.X, ALU.max), reads=[ssB[x]], writes=[stB_[y]])
            nsk4 = nvec[:, 40 + jj * 32 + hp * 4:40 + jj * 32 + hp * 4 + 4]
            STT(st_[:, 4:8], st_[:, 0:4], -1.0, nsk4, ALU.mult, ALU.min, [stB_[y], nvB], [stB_[y]])
            TT("dve", st_[:, 12:16], st_[:, 4:8], nsk4, ALU.subtract, [stB_[y], nvB], [stB_[y]])

        def st2(u):
            x = u % 2
            y = u % 4
            st_ = stat[y]
            for a in range(2):
                for ti in range(2):
                    k = a * 2 + ti
                    ACT(pbuf[x][:, a, ti, :], ssb[x][:, a, ti, :], AF.Exp, [ssB[x], stB_[y]], [pB[x], stB_[y]],
                        bias=st_[:, 4 + k:5 + k], scale=1.0, accum=st_[:, 8 + k:9 + k])
            ACT(st_[:, 16:20], st_[:, 12:16], AF.Exp, [stB_[y]], [stB_[y]])

        def st3(u):
            x = u % 2
            y = u % 4
            st_ = stat[y]
            TT("dve", st_[:, 20:24], st_[:, 8:12], st_[:, 16:20], ALU.add, [stB_[y]], [stB_[y]])
            S.add("dve", lambda e, o=st_[:, 24:28], i=st_[:, 20:24]: e.reciprocal(o, i), reads=[stB_[y]], writes=[stB_[y]])
            for k in range(4):
                TS("dve", dg[x][:, k, :], identb, st_[:, 24 + k:25 + k], ALU.mult, [cbB, stB_[y]], [dgB[x]])

        def st4(u):
            x = u % 2
            for a in range(2):
                for ti in range(2):
                    k = a * 2 + ti
                    for jt in range(2):
                        idx = k * 2 + jt
                        pb = 4 + idx // 4
                        MM(bank[pb][:, (idx % 4) * 128:(idx % 4 + 1) * 128], pbuf[x][:, a, ti, jt * 128:(jt + 1) * 128],
                           dg[x][:, k, :], True, True, [pB[x], dgB[x]], [PB[pb]])

        def st5(u):
            CP("act", pnT[:, 0:512], bank[4], [PB[4]], [pnB])
            CP("act", pnT[:, 512:1024], bank[5], [PB[5]], [pnB])

        def st6(u):
            g, hp, su = units[u]
            gk = hp // 4
            for ti in range(2):
                tt = 2 * su + ti
                t = g * 4 + tt
                ia = 0
                for a in range(2):
                    k = a * 2 + ti
                    for jt in range(2):
                        idx = k * 2 + jt
                        vi = t + jt
                        lhs = Vp[:, vi, gk, 64:192] if a == 0 else Vp[:, vi, gk, 0:128]
                        MM(bank[BO][:, tt * 128:(tt + 1) * 128], lhs, pnT[:, idx * 128:(idx + 1) * 128],
                           ia == 0, ia == 3, [vpb(vi), pnB], [PB[BO]])
                        ia += 1
            if su == 1:
                CP("act", oTg[:, hp, :], bank[BO], [PB[BO]], [oTB[hp]])
                if hp == 7:
                    for oc in range(8):
                        pb = BQ if oc % 2 == 0 else BO
                        for kc in range(8):
                            MM(bank[pb], Wo[:, kc, oc * 128:(oc + 1) * 128], oTg[:, kc, :], kc == 0, kc == 7,
                               [WoB, oTB[kc]], [PB[pb]])
                        hv = hT[:, oc, g * GS:(g + 1) * GS]
                        TT("dve", hv, bank[pb], hv, ALU.add, [PB[pb], hB[oc][g]], [hB[oc][g]])

        stages = [st0, st1, st2, st3, st4, st5, st6]
        st_q(0)
        for k in range(-2, NU + len(stages)):
            for si in reversed(range(len(stages))):
                u = k - si
                if 0 <= u < NU:
                    stages[si](u)
            if 0 <= k + 2 < NU and k + 2 > 0:
                st_q(k + 2)

    def final_store(s, with_norm=True):
        cf = Carver(UF, UFN)
        gfin = cf.take(D)
        ost = cf.take(2 * D, (2, D))
        stat = cf.take(16)
        gfB = Buf("gfin")
        osB = [Buf("ost0"), Buf("ost1")]
        fsB = Buf("fstat")
        DMA("sp", gfin, gfin_d[:, :], gfB)
        for t in range(NT):
            g = t // 4
            j = t % 2
            for half in range(2):
                pb = 2 * j + half
                for cc in range(4):
                    c = half * 4 + cc
                    TR(bank[pb][:, cc * 128:(cc + 1) * 128], hT[:, c, t * 128:(t + 1) * 128], identf[:, :],
                       [hB[c][g], constB], [PB[pb]], inc=(cc == 3))
            if with_norm:
                for half in range(2):
                    pb = 2 * j + half
                    ACT(ost[:, j, half * 512:(half + 1) * 512], bank[pb], AF.Square, [PB[pb]], [osB[j], fsB],
                        accum=stat[:, half:half + 1])
                TT("dve", stat[:, 2:3], stat[:, 0:1], stat[:, 1:2], ALU.add, [fsB], [fsB])
                ACT(stat[:, 3:4], stat[:, 2:3], AF.Ln, [fsB], [fsB], bias=EPS, scale=1.0 / D)
                ACT(stat[:, 4:5], stat[:, 3:4], AF.Exp, [fsB], [fsB], scale=-0.5)
                for half in range(2):
                    pb = 2 * j + half
                    STT(ost[:, j, half * 512:(half + 1) * 512], bank[pb], stat[:, 4:5],
                        gfin[:, half * 512:(half + 1) * 512], ALU.mult, ALU.mult, [PB[pb], fsB, gfB], [osB[j]])
            else:
                for half in range(2):
                    pb = 2 * j + half
                    CP("dve", ost[:, j, half * 512:(half + 1) * 512], bank[pb], [PB[pb]], [osB[j]])
            ob = Buf(f"out{j}")
            S.add("sp", lambda e, o=out_d[s * T + t * 128:s * T + (t + 1) * 128, :], i=ost[:, j, :]:
                  e.dma_start(out=o, in_=i), reads=[osB[j]], writes=[ob], dma_buf=ob)
            outs.append(ob)

    outs = []

    def run():
        for s in range(nseg):
            S.barrier()
            load_segment(s)
            stages = ["gla0", "mlp0", "gla1", "mlp1", "kv", "swa2", "mlp2", "swa3", "mlp3"]
            if s < nseg - 1:
                stages = stages[:5]
            for st in stages:
                if stop_after == "load":
                    break
                S.barrier()
                kind, l = st[:3], (int(st[3]) if len(st) > 3 else None)
                prefix = (s < nseg - 1)
                last = [NG - 1]
                if kind == "gla":
                    gla(l, s, out_groups=(last if (prefix and l == 1) else None))
                elif kind == "mlp":
                    mlp(l, s, preserve_kv=(l == 2), groups=(last if (prefix and l == 1) else None))
                elif st == "kv":
                    kv_phase(s, groups=(last if prefix else None))
                else:
                    swa_layer(l, s)
                if stop_after == st:
                    break
            if s == nseg - 1:
                S.barrier()
                final_store(0, with_norm=(stop_after is None))

    run()

    S.add("sp", lambda e: e.nop(), reads=outs)

    n_dma_sems = len(S.dma_bufs)
    sem_es = ExitStack()
    sems = {e: sem_es.enter_context(nc.semaphore(f"sem_{e}")) for e in ENGS}
    dma_sems = {}
    for i, b in enumerate(S.dma_bufs):
        dma_sems[id(b)] = sem_es.enter_context(nc.semaphore(f"dsem{i}"))
    print("dma semaphores:", n_dma_sems, "ops:", {e: len(S.ops[e]) for e in ENGS})
    S.prepare()
    with nc.Block() as block:
        @block.tensor
        def _(pe):
            S.emit_engine("pe", pe, sems, dma_sems)

        @block.scalar
        def _(act):
            S.emit_engine("act", act, sems, dma_sems)

        @block.vector
        def _(dve):
            S.emit_engine("dve", dve, sems, dma_sems)

        @block.gpsimd
        def _(pool):
            S.emit_engine("pool", pool, sems, dma_sems)

        @block.sync
        def _(sp):
            S.emit_engine("sp", sp, sems, dma_sems)
    sem_es.close()
    es.close()
    return nc


def _rel_bucket_band():
    BLOCK = 128
    i = np.arange(BLOCK)[:, None]
    j = np.arange(2 * BLOCK)[None, :]
    dist = i + BLOCK - j
    n = np.maximum(dist, 0)
    max_exact = 16
    large = max_exact + (np.log(np.maximum(n, 1) / max_exact) / np.log(128 / max_exact) * (32 - max_exact)).astype(np.int32)
    large = np.minimum(large, 31)
    bucket = np.where(n < max_exact, n, large).astype(np.int32)
    valid = (dist >= 0) & (dist < 128)
    return bucket, valid


def _host_consts(inp):
    f32 = np.float32
    vecs = np.zeros((128, NV), f32)

    def put(col, v):
        v = np.asarray(v, f32).reshape(-1, 128)
        for c in range(v.shape[0]):
            vecs[:, col + c] = v[c]

    for l in range(4):
        put(l * 8, inp["ln_mix"][l])
        put(32 + l * 8, inp["ln_mlp"][l])
    put(64, inp["kv_norm"])
    for l in range(2):
        put(80 + l * 4, inp["a_b_gk"][l])
        put(88 + l * 2, inp["a_onorm"][l])
    for j in range(2):
        for h in range(16):
            vecs[:, 92 + j * 16 + h] = inp["b_sinks"][j, h]
            hp, a = h // 2, h % 2
            for ti in range(2):
                vecs[:, 128 + j * 32 + hp * 4 + a * 2 + ti] = inp["b_sinks"][j, h]
    bucket, valid = _rel_bucket_band()
    biasg = np.asarray(inp["rel_table"], f32)[bucket]
    biasg = np.ascontiguousarray(biasg.transpose(0, 2, 1)).reshape(128, 16 * 256)
    maskneg = np.where(valid, 0.0, NEGM).astype(f32)
    identf = np.eye(128, dtype=f32)
    onesf = np.ones((128, 128), f32)
    p = np.arange(128)
    mask2 = ((p[:, None] // 64 == p[None, :] // 64) & (p[:, None] <= p[None, :])).astype(f32)
    scanm = np.ones((128, GS), f32)
    scanm[:, ::64] = 0.0
    gfin = np.ascontiguousarray(np.broadcast_to(np.asarray(inp["ln_final"], f32)[None, :], (128, D)))
    return dict(vecs=vecs, biasg=biasg, maskneg=maskneg, identf=identf, onesf=onesf, mask2=mask2,
                scanm=scanm, gfin=gfin)


_PROGRAM_CACHE = {}


def _get_program(nseg, stop_after=None):
    key = (nseg, stop_after)
    if key not in _PROGRAM_CACHE:
        _PROGRAM_CACHE[key] = build_program(nseg=nseg, stop_after=stop_after, dbg=stop_after is not None)
    return _PROGRAM_CACHE[key]


def kernel(x, a_w_in, a_w_gk2, a_b_gk, a_onorm, a_w_out, kv_norm, w_kv, b_w_q, b_sinks,
           b_w_out, rel_table, ln_mix, ln_mlp, w_up, w_down, ln_final):
    inp = dict(x=x, a_w_in=a_w_in, a_w_gk2=a_w_gk2, a_b_gk=a_b_gk, a_onorm=a_onorm, a_w_out=a_w_out,
               kv_norm=kv_norm, w_kv=w_kv, b_w_q=b_w_q, b_sinks=b_sinks, b_w_out=b_w_out,
               rel_table=rel_table, ln_mix=ln_mix, ln_mlp=ln_mlp, w_up=w_up, w_down=w_down, ln_final=ln_final)
    inp = {k: np.asarray(v, np.float32) for k, v in inp.items()}
    consts = _host_consts(inp)
    nc = _get_program(2)
    shared = dict(a_w_in=inp["a_w_in"], a_w_gk2=inp["a_w_gk2"], a_w_out=inp["a_w_out"], w_kv=inp["w_kv"],
                  b_w_q=inp["b_w_q"], b_w_out=inp["b_w_out"], w_up=inp["w_up"], w_down=inp["w_down"], **consts)
    ncores = 8
    in_maps = []
    for c in range(ncores):
        b, half = c // 2, c % 2
        m = dict(shared)
        xx = np.zeros((2 * T, D), np.float32)
        if half == 1:
            xx[:T] = inp["x"][b, :T]
        xx[T:] = inp["x"][b, half * T:(half + 1) * T]
        m["x"] = xx
        v = consts["vecs"].copy()
        v[:, 124] = NEGM if half == 0 else 0.0
        m["vecs"] = v
        in_maps.append(m)
    res = run_bass_kernel_spmd(nc, in_maps, core_ids=list(range(ncores)))
    out = np.empty((4, SEQ, D), np.float32)
    for c in range(ncores):
        b, half = c // 2, c % 2
        out[b, half * T:(half + 1) * T] = np.asarray(res.results[c]["out"], np.float32)
    return out
```

```python
import numpy as np
import concourse.bass as bass
import concourse.mybir as mybir
from concourse.bass_utils import run_bass_kernel_spmd

F32 = mybir.dt.float32
BF16 = mybir.dt.bfloat16
AF = mybir.ActivationFunctionType
ALU = mybir.AluOpType
AX = mybir.AxisListType

D = 1024
SEQ = 4096
T = 2048
NG = 4
GS = 512
NT = 16
DFF = 4096
EPS = 1e-6
NEGM = -30000.0
NV = 192
DBG_SWA_LEVEL = 99

class Buf:
    __slots__ = ("name", "w", "r", "sem", "cnt")

    def __init__(self, name):
        self.name = name
        self.w = None
        self.r = {}
        self.sem = None
        self.cnt = 0


class Op:
    __slots__ = ("eng", "fn", "deps", "pos", "inc_ok", "needs_inc", "tok", "dma", "dsem", "dcnt")


ENGS = ("pe", "act", "dve", "pool", "sp")


class Sched:
    def __init__(self):
        self.ops = {e: [] for e in ENGS}
        self.extra = {e: set() for e in ENGS}
        self.dma_bufs = []
        self.all_dma = []
        self.semreg = {}

    def add(self, eng, fn, reads=(), writes=(), inc_ok=True, dma_buf=None, skip_waw=False):
        op = Op()
        op.eng = eng
        op.fn = fn
        op.pos = len(self.ops[eng])
        op.inc_ok = True
        op.needs_inc = False
        op.tok = None
        op.dma = dma_buf is not None
        deps = set(self.extra[eng])
        self.extra[eng] = set()
        for b in reads:
            if b.w is not None:
                deps.add(b.w)
        for b in writes:
            if b.w is not None and not (skip_waw and b is dma_buf):
                deps.add(b.w)
            for r in b.r.values():
                deps.add(r)
        deps.discard(op)
        op.deps = deps
        if op.dma:
            sh = self.semreg.get(dma_buf.name)
            if sh is None:
                sh = Buf("sem:" + dma_buf.name)
                self.semreg[dma_buf.name] = sh
                self.dma_bufs.append(sh)
            sh.cnt += 16
            sh.w = op
            op.dsem = sh
            op.dcnt = sh.cnt
            self.all_dma.append(op)
        for b in writes:
            b.w = op
            b.r = {}
        wset = set(id(b) for b in writes)
        for b in reads:
            if id(b) in wset:
                continue
            key = ("dma", id(op)) if op.dma else eng
            b.r[key] = op
        self.ops[eng].append(op)
        return op

    def barrier(self):
        last = set()
        for e in ENGS:
            if self.ops[e]:
                last.add(self.ops[e][-1])
        for b in self.dma_bufs:
            if b.w is not None and b.w.dma:
                last.add(b.w)
        for op in self.all_dma[-64:]:
            last.add(op)
        for e in ENGS:
            self.extra[e] |= last

    def prepare(self):
        for e in ENGS:
            ops = self.ops[e]
            for op in reversed(ops):
                if not op.dma:
                    op.inc_ok = True
                    break
        nxt = {}
        for e in ENGS:
            ops = self.ops[e]
            arr = [None] * len(ops)
            cur = None
            for i in range(len(ops) - 1, -1, -1):
                if (not ops[i].dma) and ops[i].inc_ok:
                    cur = ops[i]
                arr[i] = cur
            nxt[e] = arr

        def resolve(x):
            if x.dma:
                return x
            r = nxt[x.eng][x.pos]
            assert r is not None, (x.eng, x.pos)
            return r

        for e in ENGS:
            for y in self.ops[e]:
                nd = set()
                for x in y.deps:
                    if (not x.dma) and x.eng == e:
                        if e == "pe":
                            continue
                    rx = resolve(x)
                    if not rx.dma:
                        rx.needs_inc = True
                    nd.add(rx)
                y.deps = nd
        for e in ENGS:
            n = 0
            for op in self.ops[e]:
                if (not op.dma) and op.needs_inc:
                    n += 1
                    op.tok = n

    def emit_engine(self, e, eng, sems, dma_sems):
        known = {}
        nwaits = 0
        for y in self.ops[e]:
            waits = {}
            for x in y.deps:
                if x.dma:
                    k = ("d", id(x.dsem))
                    s = dma_sems[id(x.dsem)]
                    v = x.dcnt
                else:
                    k = ("e", x.eng)
                    s = sems[x.eng]
                    v = x.tok
                if known.get(k, 0) >= v:
                    continue
                if k not in waits or waits[k][1] < v:
                    waits[k] = (s, v)
            for k, (s, v) in waits.items():
                eng.wait_ge(s, v)
                known[k] = v
                nwaits += 1
            ins = y.fn(eng)
            if y.dma:
                ins.then_inc(dma_sems[id(y.dsem)], 16)
            elif y.needs_inc:
                ins.then_inc(sems[e], 1)
        return nwaits


def build_program(nseg=2, stop_after=None, dbg=False):
    nc = bass.Bass("TRN2", target_bir_lowering=False)
    S = Sched()
    ntok = nseg * T

    def din(name, shape):
        return nc.dram_tensor(name, list(shape), F32, kind="ExternalInput").ap()

    x_d = din("x", [ntok, D])
    a_w_in = din("a_w_in", [2, D, 3088])
    a_w_gk2 = din("a_w_gk2", [2, 16, 512])
    a_w_out = din("a_w_out", [2, D, D])
    w_kv = din("w_kv", [D, 256])
    b_w_q = din("b_w_q", [2, D, D])
    b_w_out = din("b_w_out", [2, D, D])
    w_up = din("w_up", [4, D, DFF])
    w_down = din("w_down", [4, DFF, D])
    vecs_d = din("vecs", [128, NV])
    biasg_d = din("biasg", [128, 16 * 256])
    maskneg_d = din("maskneg", [128, 256])
    identf_d = din("identf", [128, 128])
    ones_d = din("onesf", [128, 128])
    mask2_d = din("mask2", [128, 128])
    scanm_d = din("scanm", [128, GS])
    gfin_d = din("gfin", [128, D])
    out_d = nc.dram_tensor("out", [T, D], F32, kind="ExternalOutput").ap()

    from contextlib import ExitStack
    es = ExitStack()

    def sb(name, shape, dt):
        return es.enter_context(nc.sbuf_tensor("s_" + name, list(shape), dt))

    hT = sb("hT", [128, 8, T], F32)
    AT = sb("AT", [128, 8, T], BF16)
    UBN = 36480
    UFN = 6720
    UB = sb("UB", [128, UBN], BF16)
    UF = sb("UF", [128, UFN], F32)
    vecs = sb("vecs", [128, NV], F32)
    nvec = sb("nvec", [128, 104], F32)
    identf = sb("identf", [128, 128], F32)
    cb = sb("cb", [128, 768], BF16)
    mask2 = sb("mask2", [128, 128], F32)
    Rst = sb("Rst", [128, 9, 256], F32)
    elast = sb("elast", [128, 8], F32)
    haloK = sb("haloK", [128, 2, 128], BF16)
    haloV = sb("haloV", [128, 2, 192], BF16)
    print("sbuf bytes remaining:", nc.sbuf_bytes_remaining)

    PS = [es.enter_context(nc.psum_tensor(f"ps{i}", [128, 512], F32)) for i in range(7)]
    PT = es.enter_context(nc.psum_tensor("pst", [128, 1024], BF16))
    PS.append(PT[:, :].bitcast(F32))
    bank = [p[:, :] if not isinstance(p, bass.AP) else p for p in PS]

    hB = [[Buf(f"h{c}_{g}") for g in range(NG)] for c in range(8)]
    AB = [[Buf(f"A{c}_{g}") for g in range(NG)] for c in range(8)]
    PB = [Buf(f"bank{i}") for i in range(8)]
    constB = Buf("consts")
    identb = cb[:, 0:128]
    onesb = cb[:, 128:256]

    def c2d(ap):
        return ap

    def MM(out, lhsT, rhs, start, stop, reads, writes, inc=None):
        inc_ok = stop if inc is None else inc
        return S.add("pe", lambda e: e.matmul(out, lhsT, rhs, start=start, stop=stop),
                     reads=reads, writes=writes, inc_ok=inc_ok)

    def TR(out, in_, ident, reads, writes, inc=True):
        return S.add("pe", lambda e: e.transpose(out, in_, ident), reads=reads, writes=writes, inc_ok=inc)

    def ACT(out, in_, func, reads, writes, bias=None, scale=None, accum=None):
        kw = {}
        if bias is not None:
            kw["bias"] = bias
        if scale is not None:
            kw["scale"] = scale
        if accum is not None:
            kw["accum_out"] = accum
        return S.add("act", lambda e: e.activation(out, in_, func, **kw), reads=reads, writes=writes)

    def TT(eng, out, in0, in1, op, reads, writes):
        return S.add(eng, lambda e: e.tensor_tensor(out, in0, in1, op), reads=reads, writes=writes)

    def TS(eng, out, in0, s1, op0, reads, writes, s2=None, op1=None):
        if op1 is None:
            return S.add(eng, lambda e: e.tensor_scalar(out, in0, s1, None, op0), reads=reads, writes=writes)
        return S.add(eng, lambda e: e.tensor_scalar(out, in0, s1, s2, op0, op1), reads=reads, writes=writes)

    def STT(out, in0, scalar, in1, op0, op1, reads, writes):
        return S.add("dve", lambda e: e.scalar_tensor_tensor(out, in0, scalar, in1, op0, op1),
                     reads=reads, writes=writes)

    def CP(eng, out, in_, reads, writes):
        if eng == "act":
            return S.add("act", lambda e: e.copy(out, in_), reads=reads, writes=writes)
        return S.add(eng, lambda e: e.tensor_copy(out, in_), reads=reads, writes=writes)

    def DMA(q, out, in_, wbuf, reads=(), skip_waw=False, extra_writes=()):
        return S.add(q, lambda e: e.dma_start(out=out, in_=in_), reads=reads,
                     writes=(wbuf,) + tuple(extra_writes), dma_buf=wbuf, skip_waw=skip_waw)

    def MEMSET(eng, ap, val, writes):
        return S.add(eng, lambda e: e.memset(ap, val), writes=writes)

    DMA("sp", vecs[:, :], vecs_d[:, :], constB)
    DMA("sp", identf[:, :], identf_d[:, :], constB, skip_waw=True)
    DMA("sp", mask2[:, :], mask2_d[:, :], constB, skip_waw=True)
    cbB = Buf("cb")
    DMA("pool", cb[:, 0:128], identf_d[:, :], cbB)
    DMA("pool", cb[:, 128:256], ones_d[:, :], cbB, skip_waw=True)
    DMA("pool", cb[:, 256:768], scanm_d[:, :], cbB, skip_waw=True)
    scanm = cb[:, 256:768]
    nvB = Buf("nvec")
    TS("dve", nvec[:, 0:8], vecs[:, 80:88], -1.0, ALU.mult, [constB], [nvB])
    TS("dve", nvec[:, 8:40], vecs[:, 92:124], -1.0, ALU.mult, [constB], [nvB])
    TS("dve", nvec[:, 40:104], vecs[:, 128:192], -1.0, ALU.mult, [constB], [nvB])
    stB = Buf("state")
    MEMSET("dve", Rst[:, :, :], 0.0, [stB])
    MEMSET("dve", elast[:, :], 1.0, [stB])
    hkB = Buf("haloK")
    hvB = Buf("haloV")
    MEMSET("dve", haloK[:, :, :], 0.0, [hkB])
    MEMSET("dve", haloV[:, :, :], 0.0, [hvB])

    def gcol(i):
        return vecs[:, i:i + 1]

    class Carver:
        def __init__(self, t, n):
            self.t = t
            self.n = n
            self.off = 0

        def take(self, n, shape=None):
            a = self.t[:, self.off:self.off + n]
            self.off += n
            assert self.off <= self.n, (self.off, self.n)
            if shape is not None:
                if len(shape) == 2:
                    a = a.rearrange("p (a b) -> p a b", a=shape[0], b=shape[1])
                elif len(shape) == 3:
                    a = a.rearrange("p (a b c) -> p a b c", a=shape[0], b=shape[1], c=shape[2])
            return a

    def rmsnorm(cf, cbf, gbase, groups=None):
        nsq = cbf.take(2 * GS, (2, GS))
        nln = cf.take(GS)
        nrs = cf.take(2 * GS, (2, GS))
        sqB = [Buf("nsq0"), Buf("nsq1")]
        lnB = Buf("nln")
        rsB = [Buf("nrs0"), Buf("nrs1")]
        for g in (range(NG) if groups is None else groups):
            pb = 0
            for c in range(8):
                j = c % 2
                ACT(nsq[:, j, :], hT[:, c, g * GS:(g + 1) * GS], AF.Square, [hB[c][g]], [sqB[j]])
                MM(bank[pb], onesb, nsq[:, j, :], c == 0, c == 7, [sqB[j], cbB], [PB[pb]])
            ACT(nln[:, :], bank[pb], AF.Ln, [PB[pb]], [lnB], bias=EPS, scale=1.0 / D)
            r = g % 2
            ACT(nrs[:, r, :], nln[:, :], AF.Exp, [lnB], [rsB[r]], scale=-0.5)
            for c in range(8):
                STT(AT[:, c, g * GS:(g + 1) * GS], hT[:, c, g * GS:(g + 1) * GS], gcol(gbase + c),
                    nrs[:, r, :], ALU.mult, ALU.mult, [hB[c][g], rsB[r], constB], [AB[c][g]])

    def load_segment(s):
        cf = Carver(UF, UFN)
        xin = cf.take(2 * D, (2, D))
        xB = [Buf("xin0"), Buf("xin1")]
        for t in range(NT):
            j = t % 2
            g = t // 4
            DMA("sp", xin[:, j, :], x_d[s * T + t * 128: s * T + (t + 1) * 128, :], xB[j])
            for half in range(2):
                pb = 2 * j + half
                for cc in range(4):
                    c = half * 4 + cc
                    TR(bank[pb][:, cc * 128:(cc + 1) * 128], xin[:, j, c * 128:(c + 1) * 128], identf[:, :],
                       [xB[j], constB], [PB[pb]], inc=(cc == 3))
                dst = hT[:, half * 4:half * 4 + 4, t * 128:(t + 1) * 128]
                src = bank[pb].rearrange("p (a b) -> p a b", a=4, b=128)
                eng = "act" if half == 0 else "dve"
                CP(eng, dst, src, [PB[pb]], [hB[half * 4 + cc][g] for cc in range(4)])

    def wload(dst, src, wbuf, first=True):
        DMA("pool", dst, src, wbuf, skip_waw=not first)

    def rows_view(w2d):
        return w2d.rearrange("(kc p) f -> p kc f", p=128)

    def mlp(l, s, preserve_kv=False, groups=None):
        groups = list(range(NG)) if groups is None else list(groups)
        cf = Carver(UF, UF_LO if preserve_kv else UFN)
        cbf = Carver(UB, UB_LO if preserve_kv else UBN)
        rmsnorm(cf, cbf, 32 + l * 8, groups)
        rt = cf.take(2 * GS, (2, GS))
        rtB = [Buf("rt0"), Buf("rt1")]
        u2 = cbf.take(4 * T, (4, T))
        u2B = [[Buf(f"u2_{c}_{g}") for g in range(NG)] for c in range(4)]
        wu = [cbf.take(8 * 512, (8, 512)) for _ in range(2)]
        wd = [cbf.take(4 * 1024, (4, 1024)) for _ in range(2)]
        wuB = [Buf("wu0"), Buf("wu1")]
        wdB = [Buf("wd0"), Buf("wd1")]
        wupv = rows_view(w_up[l])
        wdnv = rows_view(w_down[l])
        NF = DFF // 512

        def load(f):
            j = f % 2
            wload(wu[j], wupv[:, :, f * 512:(f + 1) * 512], wuB[j])
            wload(wd[j], wdnv[:, f * 4:(f + 1) * 4, :], wdB[j])

        load(0)
        setc = 0
        ri = 0
        for f in range(NF):
            j = f % 2
            if f + 1 < NF:
                load(f + 1)
            for fc in range(4):
                bs = (setc % 2) * 4
                setc += 1
                for kc in range(8):
                    for g in groups:
                        MM(bank[bs + g], wu[j][:, kc, fc * 128:(fc + 1) * 128], AT[:, kc, g * GS:(g + 1) * GS],
                           kc == 0, kc == 7, [wuB[j], AB[kc][g]], [PB[bs + g]])
                for g in groups:
                    r = ri % 2
                    ri += 1
                    ACT(rt[:, r, :], bank[bs + g], AF.Relu, [PB[bs + g]], [rtB[r]])
                    TT("dve", u2[:, fc, g * GS:(g + 1) * GS], rt[:, r, :], rt[:, r, :], ALU.mult,
                       [rtB[r]], [u2B[fc][g]])
            for oc in range(8):
                bs = (setc % 2) * 4
                setc += 1
                for k2 in range(4):
                    for g in groups:
                        MM(bank[bs + g], wd[j][:, k2, oc * 128:(oc + 1) * 128], u2[:, k2, g * GS:(g + 1) * GS],
                           k2 == 0, k2 == 3, [wdB[j], u2B[k2][g]], [PB[bs + g]])
                for g in groups:
                    hv = hT[:, oc, g * GS:(g + 1) * GS]
                    TT("dve", hv, bank[bs + g], hv, ALU.add, [PB[bs + g], hB[oc][g]], [hB[oc][g]])

    def gla(l, s, out_groups=None):
        out_groups = set(range(NG)) if out_groups is None else set(out_groups)
        cf = Carver(UF, UFN)
        cbf = Carver(UB, UBN)
        rmsnorm(cf, cbf, l * 8)
        tE = cf.take(GS)
        tL = cf.take(GS)
        Lc = cf.take(GS)
        Ae = [cf.take(GS) for _ in range(2)]
        Ai = cf.take(GS)
        rstd = cf.take(GS)
        lnv = cf.take(GS)
        sgr = cf.take(2 * GS, (2, GS))
        tEB, tLB, LcB, AiB, rsB, lnB = (Buf(n) for n in ("tE", "tL", "Lc", "Ai", "grs", "gln"))
        AeB = [Buf("Ae0"), Buf("Ae1")]
        sgrB = [Buf("sgr0"), Buf("sgr1")]
        glrT = cbf.take(T)
        wgk = cbf.take(512)
        wglr = cbf.take(8 * 16, (8, 16))
        qt = [cbf.take(GS) for _ in range(2)]
        kt = [cbf.take(GS) for _ in range(2)]
        vtok = [cbf.take(4 * 256, (4, 256)) for _ in range(2)]
        ktok = [cbf.take(4 * 128, (4, 128)) for _ in range(2)]
        sg = [cbf.take(2 * GS, (2, GS)) for _ in range(2)]
        attm = cbf.take(2 * 128, (2, 128))
        Sbf = cbf.take(8 * 256, (8, 256))
        sq = cbf.take(2 * GS, (2, GS))
        og = [cbf.take(2 * GS, (2, GS)) for _ in range(2)]
        wqkv = [cbf.take(8 * 512, (8, 512)) for _ in range(2)]
        wg = [cbf.take(8 * 256, (8, 256)) for _ in range(2)]
        wo = [cbf.take(2 * 1024, (2, 1024)) for _ in range(2)]
        glrB, wgkB, wglrB = (Buf(n) for n in ("glrT", "wgk", "wglr"))
        qtB = [Buf("qt0"), Buf("qt1")]
        ktB = [Buf("kt0"), Buf("kt1")]
        vtB = [Buf("vtok0"), Buf("vtok1")]
        ktkB = [Buf("ktok0"), Buf("ktok1")]
        sgB = [[Buf(f"sg{p}_{v}") for v in range(2)] for p in range(2)]
        atB = [Buf("att0"), Buf("att1")]
        SbB = [Buf(f"Sbf{c}") for c in range(8)]
        sqB = [Buf("gsq0"), Buf("gsq1")]
        ogB = [[Buf(f"og{p}_{v}") for v in range(2)] for p in range(2)]
        wqkvB = [Buf("wqkv0"), Buf("wqkv1")]
        wgB = [Buf("wg0"), Buf("wg1")]
        woB = [Buf("wo0"), Buf("wo1")]
        RB = [[Buf(f"R{h}_{p}") for p in range(2)] for h in range(4)]
        elB = [Buf(f"el{h}") for h in range(4)]
        for h in range(4):
            for p in range(2):
                RB[h][p].w = stB.w
            elB[h].w = stB.w

        win = rows_view(a_w_in[l])
        wov = rows_view(a_w_out[l])

        wload(wgk[0:16, :], a_w_gk2[l], wgkB)
        wload(wglr, win[:, :, 3072:3088], wglrB)

        def load_head(h):
            j = h % 2
            wload(wqkv[j][:, :, 0:128], win[:, :, h * 128:(h + 1) * 128], wqkvB[j])
            wload(wqkv[j][:, :, 128:256], win[:, :, 512 + h * 128:512 + (h + 1) * 128], wqkvB[j], first=False)
            wload(wqkv[j][:, :, 256:512], win[:, :, 1024 + h * 256:1024 + (h + 1) * 256], wqkvB[j], first=False)
            wload(wg[j], win[:, :, 2048 + h * 256:2048 + (h + 1) * 256], wgB[j])

        def load_wo(h):
            wload(wo[h % 2], wov[:, 2 * h:2 * h + 2, :], woB[h % 2])

        load_head(0)
        load_head(1)
        load_wo(0)
        load_wo(1)
        for g in range(NG):
            for kc in range(8):
                MM(bank[g][0:16, :], wglr[:, kc, :], AT[:, kc, g * GS:(g + 1) * GS], kc == 0, kc == 7,
                   [wglrB, AB[kc][g]], [PB[g]])
            CP("act", glrT[0:16, g * GS:(g + 1) * GS], bank[g][0:16, :], [PB[g]], [glrB])

        units = [(h, g) for h in range(4) for g in range(NG)]
        NU = len(units)
        rp = [0, 0, 0, 0]
        oset = [0]

        def G0(u):
            h, g = units[u]
            p = u % 2
            st = l * 4 + h
            gs = slice(g * GS, (g + 1) * GS)
            MM(bank[0], wgk[0:16, h * 128:(h + 1) * 128], glrT[0:16, gs], True, True, [wgkB, glrB], [PB[0]])
            ACT(tE, bank[0], AF.Exp, [PB[0], nvB], [tEB], bias=nvec[:, st:st + 1], scale=-1.0)
            ACT(tL, tE, AF.Ln, [tEB], [tLB], bias=1.0)

        def G0b(u):
            p = u % 2
            S.add("dve", lambda e, o=Lc, a_=scanm, b_=tL: e.tensor_tensor_scan(o, a_, b_, 0.0, ALU.mult, ALU.add),
                  reads=[tLB, cbB], writes=[LcB])
            ACT(Ae[p], Lc, AF.Exp, [LcB], [AeB[p]], scale=-1.0 / 16.0)
            ACT(Ai, Lc, AF.Exp, [LcB], [AiB], scale=1.0 / 16.0)

        def G1(u):
            h, g = units[u]
            p = u % 2
            j = h % 2
            gs = slice(g * GS, (g + 1) * GS)
            if g == 0 and h >= 1 and h + 1 < 4:
                load_head(h + 1)
            need_out = g in out_groups
            if need_out:
                for kc in range(8):
                    MM(bank[1], wqkv[j][:, kc, 0:128], AT[:, kc, gs], kc == 0, kc == 7, [wqkvB[j], AB[kc][g]], [PB[1]])
                STT(qt[p], bank[1], float(128 ** -0.5), Ae[p], ALU.mult, ALU.mult, [PB[1], AeB[p]], [qtB[p]])
                yield
            for kc in range(8):
                MM(bank[0], wqkv[j][:, kc, 128:256], AT[:, kc, gs], kc == 0, kc == 7, [wqkvB[j], AB[kc][g]], [PB[0]])
            TT("dve", kt[p], bank[0], Ai, ALU.mult, [PB[0], AiB], [ktB[p]])
            yield
            for half in range(2):
                pb = 1 - half
                for t2 in range(2):
                    tt = half * 2 + t2
                    for kc in range(8):
                        MM(bank[pb][:, t2 * 256:(t2 + 1) * 256],
                           AT[:, kc, g * GS + tt * 128:g * GS + (tt + 1) * 128], wqkv[j][:, kc, 256:512],
                           kc == 0, kc == 7, [wqkvB[j], AB[kc][g]], [PB[pb]])
                CP("act", vtok[p][:, 2 * half:2 * half + 2, :],
                   bank[pb].rearrange("p (a b) -> p a b", a=2, b=256), [PB[pb]], [vtB[p]])
                yield
            for vc in range(2):
                if not need_out:
                    break
                pb = 1 - vc
                for kc in range(8):
                    MM(bank[pb], wg[j][:, kc, vc * 128:(vc + 1) * 128], AT[:, kc, gs], kc == 0, kc == 7,
                       [wgB[j], AB[kc][g]], [PB[pb]])
                ACT(sg[p][:, vc, :], bank[pb], AF.Silu, [PB[pb]], [sgB[p][vc]])
                yield
            b1h = bank[1].bitcast(BF16)
            for tt in range(4):
                TR(b1h[:, tt * 128:(tt + 1) * 128], kt[p][:, tt * 128:(tt + 1) * 128], identb, [ktB[p], cbB], [PB[1]])
            CP("dve", ktok[p], b1h[:, 0:512].rearrange("p (a b) -> p a b", a=4, b=128), [PB[1]], [ktkB[p]])

        def G2(u):
            h, g = units[u]
            p = u % 2
            st = l * 4 + h
            R = [Rst[:, st, :], Rst[:, 8, :]]
            el = elast[:, st:st + 1]
            for c in range(8):
                tt = c // 2
                r0 = (c % 2) * 64
                pb = 2 + (c % 2)
                MM(bank[pb][:, 0:256], ktok[p][r0:r0 + 64, tt, :], vtok[p][r0:r0 + 64, tt, :], True, True,
                   [ktkB[p], vtB[p]], [PB[pb]])
                eprev = el if c == 0 else Ae[p][:, c * 64 - 1:c * 64]
                erd = [elB[h]] if c == 0 else [AeB[p]]
                r_ = rp[h]
                if g in out_groups:
                    TS("dve", Sbf[:, c, :], R[r_], eprev, ALU.mult, [RB[h][r_]] + erd, [SbB[c]])
                STT(R[1 - r_], R[r_], eprev, bank[pb][:, 0:256], ALU.mult, ALU.add,
                    [RB[h][r_], PB[pb]] + erd, [RB[h][1 - r_]])
                rp[h] = 1 - r_
                yield
            CP("dve", el, Ae[p][:, GS - 1:GS], [AeB[p]], [elB[h]])
            for tt in range(4):
                if g not in out_groups:
                    break
                ts_ = slice(tt * 128, (tt + 1) * 128)
                MM(bank[4][:, 0:128], kt[p][:, ts_], qt[p][:, ts_], True, True, [ktB[p], qtB[p]], [PB[4]])
                TT("dve", attm[:, tt % 2, :], bank[4][:, 0:128], mask2[:, :], ALU.mult, [PB[4], constB], [atB[tt % 2]])
                for vc in range(2):
                    po = 5 + vc
                    MM(bank[po][:, ts_], vtok[p][:, tt, vc * 128:(vc + 1) * 128], attm[:, tt % 2, :], True, False,
                       [vtB[p], atB[tt % 2]], [PB[po]])
                    for hf in range(2):
                        c = 2 * tt + hf
                        cs = slice(tt * 128 + hf * 64, tt * 128 + hf * 64 + 64)
                        MM(bank[po][:, cs], Sbf[:, c, vc * 128:(vc + 1) * 128], qt[p][:, cs], False, hf == 1,
                           [SbB[c], qtB[p]], [PB[po]])
                yield

        def G3(u):
            h, g = units[u]
            if g not in out_groups:
                return
            p = u % 2
            for vc in range(2):
                ACT(sq[:, vc, :], bank[5 + vc], AF.Square, [PB[5 + vc]], [sqB[vc]])
                MM(bank[2], onesb, sq[:, vc, :], vc == 0, vc == 1, [sqB[vc], cbB], [PB[2]])
            ACT(lnv, bank[2], AF.Ln, [PB[2]], [lnB], bias=EPS, scale=1.0 / 256.0)
            ACT(rstd, lnv, AF.Exp, [lnB], [rsB], scale=-0.5)

        def G3b(u):
            h, g = units[u]
            if g not in out_groups:
                return
            p = u % 2
            for vc in range(2):
                TT("dve", sgr[:, vc, :], sg[p][:, vc, :], rstd, ALU.mult, [sgB[p][vc], rsB], [sgrB[vc]])
                STT(og[p][:, vc, :], bank[5 + vc], gcol(88 + l * 2 + vc), sgr[:, vc, :], ALU.mult, ALU.mult,
                    [PB[5 + vc], sgrB[vc], constB], [ogB[p][vc]])

        def G4(u):
            h, g = units[u]
            p = u % 2
            j = h % 2
            for oc in range(8):
                if g not in out_groups:
                    break
                pb = 7 if oset[0] % 2 == 0 else 4
                oset[0] += 1
                for k2 in range(2):
                    MM(bank[pb], wo[j][:, k2, oc * 128:(oc + 1) * 128], og[p][:, k2, :], k2 == 0, k2 == 1,
                       [woB[j], ogB[p][k2]], [PB[pb]])
                hv = hT[:, oc, g * GS:(g + 1) * GS]
                TT("dve", hv, bank[pb], hv, ALU.add, [PB[pb], hB[oc][g]], [hB[oc][g]])
                yield
            if g == NG - 1 and h + 2 < 4:
                load_wo(h + 2)

        def drain(gen):
            for _ in gen:
                pass

        for k in range(NU + 5):
            if 0 <= k - 3 < NU:
                G3(k - 3)
            if 0 <= k < NU:
                G0(k)
            g4 = G4(k - 4) if 0 <= k - 4 < NU else iter(())
            g2 = G2(k - 2) if 0 <= k - 2 < NU else iter(())
            g1 = G1(k - 1) if 0 <= k - 1 < NU else iter(())
            alive = True
            rounds = 0
            did3b = False
            while alive:
                alive = False
                for gen in (g2, g1, g4):
                    try:
                        next(gen)
                        alive = True
                    except StopIteration:
                        pass
                rounds += 1
                if rounds == 3 and 0 <= k - 3 < NU:
                    G3b(k - 3)
                    did3b = True
            if not did3b and 0 <= k - 3 < NU:
                G3b(k - 3)
            if 0 <= k < NU:
                G0b(k)

    KV_ELEMS = 2 * 17 * 128 + 17 * 2 * 192
    UB_LO = UBN - KV_ELEMS
    UF_LO = UFN - 16 * 256
    swa_ctx = {}

    def kv_phase(s, groups=None):
        groups = list(range(NG)) if groups is None else list(groups)
        cf = Carver(UF, UF_LO)
        cbf = Carver(UB, UB_LO)
        rmsnorm(cf, cbf, 64, groups)
        top = Carver(UB, UBN)
        top.off = UB_LO
        KT = top.take(2 * 17 * 128, (2, 17 * 128))
        Vp = top.take(17 * 2 * 192, (17, 2, 192))
        ftop = Carver(UF, UFN)
        ftop.off = UF_LO
        biasT = ftop.take(16 * 256, (16, 256))
        biasB = Buf("biasT")
        KTB = [Buf("KT_halo")] + [Buf(f"KT{g}") for g in range(NG)]
        VpB = [Buf("Vp_halo")] + [Buf(f"Vp{g}") for g in range(NG)]
        swa_ctx.update(KT=KT, Vp=Vp, biasT=biasT, biasB=biasB, KTB=KTB, VpB=VpB)
        mk = cf.take(256)
        mkB = Buf("mk")
        DMA("sp", biasT.rearrange("p a b -> p (a b)"), biasg_d[:, :], biasB)
        DMA("sp", mk, maskneg_d[:, :], mkB)
        for hh in range(16):
            TT("dve", biasT[:, hh, :], biasT[:, hh, :], mk, ALU.add, [biasB, mkB], [biasB])
        wkd = cbf.take(8 * 256, (8, 2, 128))
        wvv = cbf.take(8 * 128, (8, 128))
        wkB, wvB = Buf("wkd"), Buf("wvv")
        wkvv = rows_view(w_kv)
        first = True
        for gk in range(2):
            for d2 in range(2):
                wload(wkd[:, :, gk, d2 * 64:(d2 + 1) * 64], wkvv[:, :, gk * 64:(gk + 1) * 64], wkB, first=first)
                first = False
        wload(wvv, wkvv[:, :, 128:256], wvB)
        MEMSET("pool", Vp[:, :, :, :], 0.0, VpB)
        CP("pool", KT[:, :, 0:128], haloK[:, :, :], [hkB], [KTB[0]])
        CP("pool", Vp[:, 0, :, :], haloV[:, :, :], [hvB], [VpB[0]])
        for gk in range(2):
            for kc in range(8):
                for g in groups:
                    MM(bank[g], wkd[:, kc, gk, :], AT[:, kc, g * GS:(g + 1) * GS], kc == 0, kc == 7,
                       [wkB, AB[kc][g]], [PB[g]])
            for g in groups:
                CP("act", KT[:, gk, 128 + g * GS:128 + (g + 1) * GS], bank[g], [PB[g]], [KTB[1 + g]])
        for g in groups:
            pb = 4 + (g % 2)
            for tt in range(4):
                for kc in range(8):
                    MM(bank[pb][:, tt * 128:(tt + 1) * 128], AT[:, kc, g * GS + tt * 128:g * GS + (tt + 1) * 128],
                       wvv[:, kc, :], kc == 0, kc == 7, [wvB, AB[kc][g]], [PB[pb]], inc=(kc == 7 and tt == 3))
            CP("act", Vp[:, 1 + 4 * g:5 + 4 * g, :, 64:128],
               bank[pb].rearrange("p (a b c) -> p a b c", a=4, b=2, c=64), [PB[pb]], [VpB[1 + g]])
        CP("pool", haloK[:, :, :], KT[:, :, 16 * 128:17 * 128], [KTB[NG]], [hkB])
        CP("pool", haloV[:, :, :], Vp[:, 16, :, :], [VpB[NG]], [hvB])

    def swa_layer(l, s):
        jj = l - 2
        KT, Vp, biasT, biasB = swa_ctx["KT"], swa_ctx["Vp"], swa_ctx["biasT"], swa_ctx["biasB"]
        KTB, VpB = swa_ctx["KTB"], swa_ctx["VpB"]
        cf = Carver(UF, UF_LO)
        cbf = Carver(UB, UB_LO)
        rmsnorm(cf, cbf, l * 8)
        S.barrier()
        cf = Carver(UF, UF_LO)
        cbf = Carver(UB, UB_LO)
        ssb = [cf.take(1024, (2, 2, 256)) for _ in range(2)]
        stat = [cf.take(32) for _ in range(4)]
        Wq = cbf.take(8 * 1024, (8, 1024))
        Wo = cbf.take(8 * 1024, (8, 1024))
        qh = cbf.take(2 * GS, (2, GS))
        oTg = cbf.take(8 * GS, (8, GS))
        pbuf = [cbf.take(1024, (2, 2, 256)) for _ in range(2)]
        dg = [cbf.take(512, (4, 128)) for _ in range(2)]
        pnT = cbf.take(1024)
        ssB = [Buf("ss0"), Buf("ss1")]
        WqB, WoB = Buf("Wq"), Buf("Wo")
        qhB = [Buf("qh0"), Buf("qh1")]
        oTB = [Buf(f"oT{c}") for c in range(8)]
        pB = [Buf("p0"), Buf("p1")]
        pnB = Buf("pn")
        dgB = [Buf("dg0"), Buf("dg1")]
        stB_ = [Buf(f"stat{i}") for i in range(4)]
        for hf in range(2):
            wload(Wq[:, :, hf * 512:(hf + 1) * 512], rows_view(b_w_q[jj])[:, :, hf * 512:(hf + 1) * 512], WqB, first=(hf == 0))
        for hf in range(2):
            wload(Wo[:, :, hf * 512:(hf + 1) * 512], rows_view(b_w_out[jj])[:, :, hf * 512:(hf + 1) * 512], WoB, first=(hf == 0))
        units = [(g, hp, su) for g in range(NG) for hp in range(8) for su in range(2)]
        NU = len(units)
        BQ, BO = 7, 6

        def ktb(t):
            r = [KTB[1 + (t // 4)]]
            r.append(KTB[0] if t == 0 else KTB[1 + ((t - 1) // 4)])
            return r

        def vpb(i):
            return VpB[0] if i == 0 else VpB[1 + (i - 1) // 4]

        def st_q(u):
            g, hp, su = units[u]
            if su != 0:
                return
            qb = hp % 2
            for kc in range(8):
                MM(bank[BQ], Wq[:, kc, hp * 128:(hp + 1) * 128], AT[:, kc, g * GS:(g + 1) * GS],
                   kc == 0, kc == 7, [WqB, AB[kc][g]], [PB[BQ]])
            S.add("act", lambda e, o=qh[:, qb, :], i=bank[BQ]: e.mul(o, i, 0.125), reads=[PB[BQ]], writes=[qhB[qb]])

        def st0(u):
            g, hp, su = units[u]
            x = u % 2
            gk = hp // 4
            qb = hp % 2
            for ti in range(2):
                tt = 2 * su + ti
                t = g * 4 + tt
                for a in range(2):
                    r0 = a * 64
                    MM(bank[2 * x + a][:, ti * 256:(ti + 1) * 256], qh[r0:r0 + 64, qb, tt * 128:(tt + 1) * 128],
                       KT[r0:r0 + 64, gk, t * 128:(t + 2) * 128], True, True, [qhB[qb]] + ktb(t), [PB[2 * x + a]])

        def st1(u):
            g, hp, su = units[u]
            x = u % 2
            for a in range(2):
                for ti in range(2):
                    TT("dve", ssb[x][:, a, ti, :], bank[2 * x + a][:, ti * 256:(ti + 1) * 256],
                       biasT[:, 2 * hp + a, :], ALU.add, [PB[2 * x + a], biasB], [ssB[x]])
            if g == 0 and su == 0:
                for a in range(2):
                    TS("dve", ssb[x][:, a, 0, 0:128], ssb[x][:, a, 0, 0:128], vecs[:, 124:125], ALU.add,
                       [ssB[x], constB], [ssB[x]])
            y = u % 4
            st_ = stat[y]
            S.add("dve", lambda e, o=st_[:, 0:4], i=ssb[x].rearrange("p a t k -> p (a t) k"):
                  e.tensor_reduce(o, i, AX.X, ALU.max), reads=[ssB[x]], writes=[stB_[y]])
            nsk4 = nvec[:, 40 + jj * 32 + hp * 4:40 + jj * 32 + hp * 4 + 4]
            STT(st_[:, 4:8], st_[:, 0:4], -1.0, nsk4, ALU.mult, ALU.min, [stB_[y], nvB], [stB_[y]])
            TT("dve", st_[:, 12:16], st_[:, 4:8], nsk4, ALU.subtract, [stB_[y], nvB], [stB_[y]])

        def st2(u):
            x = u % 2
            y = u % 4
            st_ = stat[y]
            for a in range(2):
                for ti in range(2):
                    k = a * 2 + ti
                    ACT(pbuf[x][:, a, ti, :], ssb[x][:, a, ti, :], AF.Exp, [ssB[x], stB_[y]], [pB[x], stB_[y]],
                        bias=st_[:, 4 + k:5 + k], scale=1.0, accum=st_[:, 8 + k:9 + k])
            ACT(st_[:, 16:20], st_[:, 12:16], AF.Exp, [stB_[y]], [stB_[y]])

        def st3(u):
            x = u % 2
            y = u % 4
            st_ = stat[y]
            TT("dve", st_[:, 20:24], st_[:, 8:12], st_[:, 16:20], ALU.add, [stB_[y]], [stB_[y]])
            S.add("dve", lambda e, o=st_[:, 24:28], i=st_[:, 20:24]: e.reciprocal(o, i), reads=[stB_[y]], writes=[stB_[y]])
            for k in range(4):
                TS("dve", dg[x][:, k, :], identb, st_[:, 24 + k:25 + k], ALU.mult, [cbB, stB_[y]], [dgB[x]])

        def st4(u):
            x = u % 2
            for a in range(2):
                for ti in range(2):
                    k = a * 2 + ti
                    for jt in range(2):
                        idx = k * 2 + jt
                        pb = 4 + idx // 4
                        MM(bank[pb][:, (idx % 4) * 128:(idx % 4 + 1) * 128], pbuf[x][:, a, ti, jt * 128:(jt + 1) * 128],
                           dg[x][:, k, :], True, True, [pB[x], dgB[x]], [PB[pb]])

        def st5(u):
            CP("act", pnT[:, 0:512], bank[4], [PB[4]], [pnB])
            CP("act", pnT[:, 512:1024], bank[5], [PB[5]], [pnB])

        def st6(u):
            g, hp, su = units[u]
            gk = hp // 4
            for ti in range(2):
                tt = 2 * su + ti
                t = g * 4 + tt
                ia = 0
                for a in range(2):
                    k = a * 2 + ti
                    for jt in range(2):
                        idx = k * 2 + jt
                        vi = t + jt
                        lhs = Vp[:, vi, gk, 64:192] if a == 0 else Vp[:, vi, gk, 0:128]
                        MM(bank[BO][:, tt * 128:(tt + 1) * 128], lhs, pnT[:, idx * 128:(idx + 1) * 128],
                           ia == 0, ia == 3, [vpb(vi), pnB], [PB[BO]])
                        ia += 1
            if su == 1:
                CP("act", oTg[:, hp, :], bank[BO], [PB[BO]], [oTB[hp]])
                if hp == 7:
                    for oc in range(8):
                        pb = BQ if oc % 2 == 0 else BO
                        for kc in range(8):
                            MM(bank[pb], Wo[:, kc, oc * 128:(oc + 1) * 128], oTg[:, kc, :], kc == 0, kc == 7,
                               [WoB, oTB[kc]], [PB[pb]])
                        hv = hT[:, oc, g * GS:(g + 1) * GS]
                        TT("dve", hv, bank[pb], hv, ALU.add, [PB[pb], hB[oc][g]], [hB[oc][g]])

        stages = [st0, st1, st2, st3, st4, st5, st6]
        st_q(0)
        for k in range(-2, NU + len(stages)):
            for si in reversed(range(len(stages))):
                u = k - si
                if 0 <= u < NU:
                    stages[si](u)
            if 0 <= k + 2 < NU and k + 2 > 0:
                st_q(k + 2)

    def final_store(s, with_norm=True):
        cf = Carver(UF, UFN)
        gfin = cf.take(D)
        ost = cf.take(2 * D, (2, D))
        stat = cf.take(16)
        gfB = Buf("gfin")
        osB = [Buf("ost0"), Buf("ost1")]
        fsB = Buf("fstat")
        DMA("sp", gfin, gfin_d[:, :], gfB)
        for t in range(NT):
            g = t // 4
            j = t % 2
            for half in range(2):
                pb = 2 * j + half
                for cc in range(4):
                    c = half * 4 + cc
                    TR(bank[pb][:, cc * 128:(cc + 1) * 128], hT[:, c, t * 128:(t + 1) * 128], identf[:, :],
                       [hB[c][g], constB], [PB[pb]], inc=(cc == 3))
            if with_norm:
                for half in range(2):
                    pb = 2 * j + half
                    ACT(ost[:, j, half * 512:(half + 1) * 512], bank[pb], AF.Square, [PB[pb]], [osB[j], fsB],
                        accum=stat[:, half:half + 1])
                TT("dve", stat[:, 2:3], stat[:, 0:1], stat[:, 1:2], ALU.add, [fsB], [fsB])
                ACT(stat[:, 3:4], stat[:, 2:3], AF.Ln, [fsB], [fsB], bias=EPS, scale=1.0 / D)
                ACT(stat[:, 4:5], stat[:, 3:4], AF.Exp, [fsB], [fsB], scale=-0.5)
                for half in range(2):
                    pb = 2 * j + half
                    STT(ost[:, j, half * 512:(half + 1) * 512], bank[pb], stat[:, 4:5],
                        gfin[:, half * 512:(half + 1) * 512], ALU.mult, ALU.mult, [PB[pb], fsB, gfB], [osB[j]])
            else:
                for half in range(2):
                    pb = 2 * j + half
                    CP("dve", ost[:, j, half * 512:(half + 1) * 512], bank[pb], [PB[pb]], [osB[j]])
            ob = Buf(f"out{j}")
            S.add("sp", lambda e, o=out_d[s * T + t * 128:s * T + (t + 1) * 128, :], i=ost[:, j, :]:
                  e.dma_start(out=o, in_=i), reads=[osB[j]], writes=[ob], dma_buf=ob)
            outs.append(ob)

    outs = []

    def run():
        for s in range(nseg):
            S.barrier()
            load_segment(s)
            stages = ["gla0", "mlp0", "gla1", "mlp1", "kv", "swa2", "mlp2", "swa3", "mlp3"]
            if s < nseg - 1:
                stages = stages[:5]
            for st in stages:
                if stop_after == "load":
                    break
                S.barrier()
                kind, l = st[:3], (int(st[3]) if len(st) > 3 else None)
                prefix = (s < nseg - 1)
                last = [NG - 1]
                if kind == "gla":
                    gla(l, s, out_groups=(last if (prefix and l == 1) else None))
                elif kind == "mlp":
                    mlp(l, s, preserve_kv=(l == 2), groups=(last if (prefix and l == 1) else None))
                elif st == "kv":
                    kv_phase(s, groups=(last if prefix else None))
                else:
                    swa_layer(l, s)
                if stop_after == st:
                    break
            if s == nseg - 1:
                S.barrier()
                final_store(0, with_norm=(stop_after is None))

    run()

    S.add("sp", lambda e: e.nop(), reads=outs)

    n_dma_sems = len(S.dma_bufs)
    sem_es = ExitStack()
    sems = {e: sem_es.enter_context(nc.semaphore(f"sem_{e}")) for e in ENGS}
    dma_sems = {}
    for i, b in enumerate(S.dma_bufs):
        dma_sems[id(b)] = sem_es.enter_context(nc.semaphore(f"dsem{i}"))
    print("dma semaphores:", n_dma_sems, "ops:", {e: len(S.ops[e]) for e in ENGS})
    S.prepare()
    with nc.Block() as block:
        @block.tensor
        def _(pe):
            S.emit_engine("pe", pe, sems, dma_sems)

        @block.scalar
        def _(act):
            S.emit_engine("act", act, sems, dma_sems)

        @block.vector
        def _(dve):
            S.emit_engine("dve", dve, sems, dma_sems)

        @block.gpsimd
        def _(pool):
            S.emit_engine("pool", pool, sems, dma_sems)

        @block.sync
        def _(sp):
            S.emit_engine("sp", sp, sems, dma_sems)
    sem_es.close()
    es.close()
    return nc


def _rel_bucket_band():
    BLOCK = 128
    i = np.arange(BLOCK)[:, None]
    j = np.arange(2 * BLOCK)[None, :]
    dist = i + BLOCK - j
    n = np.maximum(dist, 0)
    max_exact = 16
    large = max_exact + (np.log(np.maximum(n, 1) / max_exact) / np.log(128 / max_exact) * (32 - max_exact)).astype(np.int32)
    large = np.minimum(large, 31)
    bucket = np.where(n < max_exact, n, large).astype(np.int32)
    valid = (dist >= 0) & (dist < 128)
    return bucket, valid


def _host_consts(inp):
    f32 = np.float32
    vecs = np.zeros((128, NV), f32)

    def put(col, v):
        v = np.asarray(v, f32).reshape(-1, 128)
        for c in range(v.shape[0]):
            vecs[:, col + c] = v[c]

    for l in range(4):
        put(l * 8, inp["ln_mix"][l])
        put(32 + l * 8, inp["ln_mlp"][l])
    put(64, inp["kv_norm"])
    for l in range(2):
        put(80 + l * 4, inp["a_b_gk"][l])
        put(88 + l * 2, inp["a_onorm"][l])
    for j in range(2):
        for h in range(16):
            vecs[:, 92 + j * 16 + h] = inp["b_sinks"][j, h]
            hp, a = h // 2, h % 2
            for ti in range(2):
                vecs[:, 128 + j * 32 + hp * 4 + a * 2 + ti] = inp["b_sinks"][j, h]
    bucket, valid = _rel_bucket_band()
    biasg = np.asarray(inp["rel_table"], f32)[bucket]
    biasg = np.ascontiguousarray(biasg.transpose(0, 2, 1)).reshape(128, 16 * 256)
    maskneg = np.where(valid, 0.0, NEGM).astype(f32)
    identf = np.eye(128, dtype=f32)
    onesf = np.ones((128, 128), f32)
    p = np.arange(128)
    mask2 = ((p[:, None] // 64 == p[None, :] // 64) & (p[:, None] <= p[None, :])).astype(f32)
    scanm = np.ones((128, GS), f32)
    scanm[:, ::64] = 0.0
    gfin = np.ascontiguousarray(np.broadcast_to(np.asarray(inp["ln_final"], f32)[None, :], (128, D)))
    return dict(vecs=vecs, biasg=biasg, maskneg=maskneg, identf=identf, onesf=onesf, mask2=mask2,
                scanm=scanm, gfin=gfin)


_PROGRAM_CACHE = {}


def _get_program(nseg, stop_after=None):
    key = (nseg, stop_after)
    if key not in _PROGRAM_CACHE:
        _PROGRAM_CACHE[key] = build_program(nseg=nseg, stop_after=stop_after, dbg=stop_after is not None)
    return _PROGRAM_CACHE[key]


def kernel(x, a_w_in, a_w_gk2, a_b_gk, a_onorm, a_w_out, kv_norm, w_kv, b_w_q, b_sinks,
           b_w_out, rel_table, ln_mix, ln_mlp, w_up, w_down, ln_final):
    inp = dict(x=x, a_w_in=a_w_in, a_w_gk2=a_w_gk2, a_b_gk=a_b_gk, a_onorm=a_onorm, a_w_out=a_w_out,
               kv_norm=kv_norm, w_kv=w_kv, b_w_q=b_w_q, b_sinks=b_sinks, b_w_out=b_w_out,
               rel_table=rel_table, ln_mix=ln_mix, ln_mlp=ln_mlp, w_up=w_up, w_down=w_down, ln_final=ln_final)
    inp = {k: np.asarray(v, np.float32) for k, v in inp.items()}
    consts = _host_consts(inp)
    nc = _get_program(2)
    shared = dict(a_w_in=inp["a_w_in"], a_w_gk2=inp["a_w_gk2"], a_w_out=inp["a_w_out"], w_kv=inp["w_kv"],
                  b_w_q=inp["b_w_q"], b_w_out=inp["b_w_out"], w_up=inp["w_up"], w_down=inp["w_down"], **consts)
    ncores = 8
    in_maps = []
    for c in range(ncores):
        b, half = c // 2, c % 2
        m = dict(shared)
        xx = np.zeros((2 * T, D), np.float32)
        if half == 1:
            xx[:T] = inp["x"][b, :T]
        xx[T:] = inp["x"][b, half * T:(half + 1) * T]
        m["x"] = xx
        v = consts["vecs"].copy()
        v[:, 124] = NEGM if half == 0 else 0.0
        m["vecs"] = v
        in_maps.append(m)
    res = run_bass_kernel_spmd(nc, in_maps, core_ids=list(range(ncores)))
    out = np.empty((4, SEQ, D), np.float32)
    for c in range(ncores):
        b, half = c // 2, c % 2
        out[b, half * T:(half + 1) * T] = np.asarray(res.results[c]["out"], np.float32)
    return out
```
